# Optimizing a Trainium2 kernel written in Bass

```python
import math
import jax, jax.numpy as jnp
from jax import lax
import numpy as np

D_MODEL = 2048
BATCH = 16
SEQ = 2048
DEPTH = 1

RMS_EPS = 1e-6
RWKV_WIDTH = D_MODEL // 2
RWKV_HEAD = 64
RWKV_HEADS = RWKV_WIDTH // RWKV_HEAD
DECAY_RANK = 64
AAA_RANK = 64
GATE_RANK = 160
RWKV_GN_EPS = 64e-5
S5_WIDTH = D_MODEL // 2
S5_GROUP = 16
S5_GROUPS = S5_WIDTH // S5_GROUP
S5_STATE = 64
DT_MIN = 1e-3
DT_MAX = 1e-1
N_GROUPS = 4
EXPERTS_PER_GROUP = 8
N_EXPERTS = N_GROUPS * EXPERTS_PER_GROUP
TOP_K_INNER = 2
D_EXPERT = D_MODEL // 4
RWKV_SPLITS = [RWKV_WIDTH, 2 * RWKV_WIDTH, 3 * RWKV_WIDTH, 3 * RWKV_WIDTH + DECAY_RANK, 3 * RWKV_WIDTH + DECAY_RANK + AAA_RANK]
RWKV_COLS = 3 * RWKV_WIDTH + DECAY_RANK + AAA_RANK + GATE_RANK
IN_SPLITS = [RWKV_COLS, RWKV_COLS + S5_WIDTH, RWKV_COLS + S5_WIDTH + D_MODEL]
IN_COLS = RWKV_COLS + S5_WIDTH + 2 * D_MODEL

kernel_name = "hybrid_rwkv7_s5_hmoe_block"


def rms_norm(x, g):
    xf = x.astype(jnp.float32)
    y = xf * lax.rsqrt(jnp.mean(xf * xf, axis=-1, keepdims=True) + RMS_EPS)
    return (y * g.astype(jnp.float32)).astype(x.dtype)


def token_shift(p, mu):
    prev = jnp.pad(p, ((0, 0), (1, 0), (0, 0)))[:, :-1]
    return p + (prev - p) * mu


def rwkv7_recurrence(r, w, k, v, a, b):
    Bn, T, H, N = r.shape
    seq = tuple(jnp.moveaxis(t, 1, 0) for t in (r, w, k, v, a, b))

    def step(S, inp):
        rt, wt, kt, vt, at, bt = inp
        sa = jnp.einsum('bhij,bhj->bhi', S, at)
        S = S * wt[:, :, None, :] + sa[..., None] * bt[:, :, None, :] + vt[..., None] * kt[:, :, None, :]
        y = jnp.einsum('bhij,bhj->bhi', S, rt)
        return S, y

    S0 = jnp.zeros((Bn, H, N, N), jnp.float32)
    _, ys = lax.scan(step, S0, seq)
    return jnp.moveaxis(ys, 0, 1)


def rwkv7_branch(p, mu, w0, w_up, a0, a_up, g_up, k_k, k_a, r_k, ln_w, ln_b, w_out):
    f32 = jnp.float32
    Bn, T, _ = p.shape
    heads = lambda t: t.reshape(Bn, T, RWKV_HEADS, RWKV_HEAD)
    p = token_shift(p, mu)
    r, k, v, xw, xa, xg = jnp.split(p, RWKV_SPLITS, axis=-1)
    w_log = -jax.nn.softplus(-(w0 + jnp.tanh(xw) @ w_up).astype(f32)) - 0.5
    decay = jnp.exp(-jnp.exp(w_log))
    a = jax.nn.sigmoid((a0 + xa @ a_up).astype(f32))
    g = jax.nn.sigmoid(xg) @ g_up
    k = k.astype(f32)
    kk = heads(k * k_k.astype(f32))
    kk = kk / jnp.maximum(jnp.linalg.norm(kk, axis=-1, keepdims=True), 1e-12)
    k = k * (1.0 + (a - 1.0) * k_a.astype(f32))
    rh, kh, vh = heads(r.astype(f32)), heads(k), heads(v.astype(f32))
    y = rwkv7_recurrence(rh, heads(decay), kh, vh, -kk, kk * heads(a))
    mean = jnp.mean(y, axis=-1, keepdims=True)
    var = jnp.mean(jnp.square(y - mean), axis=-1, keepdims=True)
    y = ((y - mean) * lax.rsqrt(var + RWKV_GN_EPS)).reshape(Bn, T, RWKV_WIDTH)
    y = y * ln_w.astype(f32) + ln_b.astype(f32)
    bonus = jnp.sum(rh * kh * r_k.astype(f32), axis=-1, keepdims=True) * vh
    y = y + bonus.reshape(Bn, T, RWKV_WIDTH)
    return (y * g.astype(f32)).astype(p.dtype) @ w_out


def s5_branch(u, lam_re, lam_im, log_step, b_re, b_im, c_re, c_im, d_skip, w_glu_v, w_glu_g):
    f32 = jnp.float32
    Bn, T, _ = u.shape
    uf = u.astype(f32).reshape(Bn, T, S5_GROUPS, S5_GROUP)
    lam = lax.complex(lam_re.astype(f32), lam_im.astype(f32))
    step = jnp.exp(log_step.astype(f32))[:, None]
    a_bar = jnp.exp(lam * step)
    b_bar = ((a_bar - 1.0) / lam)[..., None] * lax.complex(b_re.astype(f32), b_im.astype(f32))
    bu = lax.complex(jnp.einsum('btgh,gph->btgp', uf, jnp.real(b_bar)),
                     jnp.einsum('btgh,gph->btgp', uf, jnp.imag(b_bar)))
    a_elems = jnp.broadcast_to(a_bar, bu.shape)

    def combine(left, right):
        a_l, b_l = left
        a_r, b_r = right
        return a_r * a_l, a_r * b_l + b_r

    _, xs = lax.associative_scan(combine, (a_elems, bu), axis=1)
    y = (jnp.einsum('btgp,ghp->btgh', jnp.real(xs), c_re.astype(f32))
         - jnp.einsum('btgp,ghp->btgh', jnp.imag(xs), c_im.astype(f32))
         + d_skip.astype(f32).reshape(S5_GROUPS, S5_GROUP) * uf)
    z = jax.nn.gelu(y.reshape(Bn, T, S5_WIDTH)).astype(u.dtype)
    return (z @ w_glu_v) * jax.nn.sigmoid(z @ w_glu_g)


def hierarchical_moe(h, wr_group, br_group, wr_expert, br_expert, w_gate, w_up, w_down):
    f32 = jnp.float32
    Bn, T, D = h.shape
    n_tok = Bn * T
    ht = h.reshape(n_tok, D)
    group_prob = jax.nn.softmax((ht @ wr_group).astype(f32) + br_group.astype(f32), axis=-1)
    g_prob, g_idx = lax.top_k(group_prob, 1)
    expert_logits = ((ht @ wr_expert).astype(f32) + br_expert.astype(f32)).reshape(n_tok, N_GROUPS, EXPERTS_PER_GROUP)
    sel = expert_logits[jnp.arange(n_tok), g_idx[:, 0]]
    top_logit, top_idx = lax.top_k(sel, TOP_K_INNER)
    weights = jax.nn.softmax(top_logit, axis=-1) * g_prob
    expert_id = g_idx * EXPERTS_PER_GROUP + top_idx
    gates = jnp.sum(jax.nn.one_hot(expert_id, N_EXPERTS, dtype=f32) * weights[..., None], axis=1)
    out = jnp.zeros((n_tok, D), f32)
    for e in range(N_EXPERTS):
        hid = jax.nn.silu(ht @ w_gate[e]) * (ht @ w_up[e])
        out = out + gates[:, e:e + 1] * (hid @ w_down[e]).astype(f32)
    return out.astype(h.dtype).reshape(Bn, T, D)


def setup_inputs(seed: int = 0) -> dict:
    key = jax.random.key(seed)
    ks = jax.random.split(key, 40)
    f32 = jnp.float32
    L, D, W, H, N = DEPTH, D_MODEL, RWKV_WIDTH, RWKV_HEADS, RWKV_HEAD
    G, Hs, P = S5_GROUPS, S5_GROUP, S5_STATE

    def nrm(i, shape, scale):
        return scale * jax.random.normal(ks[i], shape, f32)

    ramp = jnp.arange(W, dtype=f32) / (W - 1)
    w0_init = -6.5 + 5.0 * ramp ** 0.85
    lam_im_init = jnp.pi * jnp.arange(P, dtype=f32)
    return {
        "x": nrm(0, (BATCH, SEQ, D), 1.0),
        "norm_mix_g": 1.0 + nrm(1, (L, D), 0.02),
        "w_in": nrm(2, (L, D, IN_COLS), D ** -0.5),
        "rwkv_mu": 0.5 + nrm(3, (L, RWKV_COLS), 0.1),
        "rwkv_w0": w0_init + nrm(4, (L, W), 0.05),
        "rwkv_w_up": nrm(5, (L, DECAY_RANK, W), 0.1 * DECAY_RANK ** -0.5),
        "rwkv_a0": nrm(6, (L, W), 0.1),
        "rwkv_a_up": nrm(7, (L, AAA_RANK, W), 0.1 * AAA_RANK ** -0.5),
        "rwkv_g_up": nrm(8, (L, GATE_RANK, W), GATE_RANK ** -0.5),
        "rwkv_k_k": 0.85 + nrm(9, (L, W), 0.02),
        "rwkv_k_a": 1.0 + nrm(10, (L, W), 0.02),
        "rwkv_r_k": nrm(11, (L, H, N), 0.1),
        "rwkv_ln_w": 1.0 + nrm(12, (L, W), 0.02),
        "rwkv_ln_b": nrm(13, (L, W), 0.02),
        "rwkv_w_out": nrm(14, (L, W, D), W ** -0.5),
        "s5_lam_re": -0.5 + nrm(15, (L, G, P), 0.01),
        "s5_lam_im": lam_im_init + nrm(16, (L, G, P), 0.01),
        "s5_log_step": jax.random.uniform(ks[17], (L, G), f32, math.log(DT_MIN), math.log(DT_MAX)),
        "s5_b_re": nrm(18, (L, G, P, Hs), (2 * Hs) ** -0.5),
        "s5_b_im": nrm(19, (L, G, P, Hs), (2 * Hs) ** -0.5),
        "s5_c_re": nrm(20, (L, G, Hs, P), (2 * P) ** -0.5),
        "s5_c_im": nrm(21, (L, G, Hs, P), (2 * P) ** -0.5),
        "s5_d": nrm(22, (L, S5_WIDTH), 1.0),
        "s5_w_glu_v": nrm(23, (L, S5_WIDTH, D), S5_WIDTH ** -0.5),
        "s5_w_glu_g": nrm(24, (L, S5_WIDTH, D), S5_WIDTH ** -0.5),
        "w_out": nrm(25, (L, D, D), D ** -0.5),
        "norm_ffn_g": 1.0 + nrm(26, (L, D), 0.02),
        "router_group_w": nrm(27, (L, D, N_GROUPS), D ** -0.5),
        "router_group_b": nrm(28, (L, N_GROUPS), 0.01),
        "router_expert_w": nrm(29, (L, D, N_EXPERTS), D ** -0.5),
        "router_expert_b": nrm(30, (L, N_EXPERTS), 0.01),
        "moe_w_gate": nrm(31, (L, N_EXPERTS, D, D_EXPERT), D ** -0.5),
        "moe_w_up": nrm(32, (L, N_EXPERTS, D, D_EXPERT), D ** -0.5),
        "moe_w_down": nrm(33, (L, N_EXPERTS, D_EXPERT, D), D_EXPERT ** -0.5),
        "norm_final_g": 1.0 + nrm(34, (D,), 0.02),
    }


def reference(x, norm_mix_g, w_in, rwkv_mu, rwkv_w0, rwkv_w_up, rwkv_a0, rwkv_a_up, rwkv_g_up,
              rwkv_k_k, rwkv_k_a, rwkv_r_k, rwkv_ln_w, rwkv_ln_b, rwkv_w_out,
              s5_lam_re, s5_lam_im, s5_log_step, s5_b_re, s5_b_im, s5_c_re, s5_c_im, s5_d,
              s5_w_glu_v, s5_w_glu_g, w_out, norm_ffn_g, router_group_w, router_group_b,
              router_expert_w, router_expert_b, moe_w_gate, moe_w_up, moe_w_down, norm_final_g):
    for l in range(DEPTH):
        h = rms_norm(x, norm_mix_g[l])
        proj = h @ w_in[l]
        p_rwkv, u, gate_a, gate_b = jnp.split(proj, IN_SPLITS, axis=-1)
        y_a = rwkv7_branch(p_rwkv, rwkv_mu[l], rwkv_w0[l], rwkv_w_up[l], rwkv_a0[l], rwkv_a_up[l],
                           rwkv_g_up[l], rwkv_k_k[l], rwkv_k_a[l], rwkv_r_k[l], rwkv_ln_w[l],
                           rwkv_ln_b[l], rwkv_w_out[l])
        y_b = s5_branch(u, s5_lam_re[l], s5_lam_im[l], s5_log_step[l], s5_b_re[l], s5_b_im[l],
                        s5_c_re[l], s5_c_im[l], s5_d[l], s5_w_glu_v[l], s5_w_glu_g[l])
        mixed = jax.nn.sigmoid(gate_a) * y_a + jax.nn.sigmoid(gate_b) * y_b
        x = x + mixed @ w_out[l]
        x = x + hierarchical_moe(rms_norm(x, norm_ffn_g[l]), router_group_w[l], router_group_b[l],
                                 router_expert_w[l], router_expert_b[l], moe_w_gate[l],
                                 moe_w_up[l], moe_w_down[l])
    return rms_norm(x, norm_final_g)
```

```python
import math
from contextlib import ExitStack
import numpy as np
import concourse.bass as bass
import concourse.mybir as mybir
from concourse.bass_utils import run_bass_kernel_spmd

F32 = mybir.dt.float32
BF16 = mybir.dt.bfloat16
I32 = mybir.dt.int32
AF = mybir.ActivationFunctionType
ALU = mybir.AluOpType
AX = mybir.AxisListType

D = 2048
NCOL = 8480
SEQ = 2048
NB = 2
TM = 128
NTM = SEQ // TM
NE = 32
DE = 512
TF = 512
NTF = NB * SEQ // TF
RMS_EPS = 1e-6
GN_EPS = 64e-5
DEC_K = math.exp(-0.5)
TWO_PI = 2.0 * math.pi


class Buf:
    __slots__ = ("name", "w", "r", "semcnt", "sem")

    def __init__(self, name):
        self.name = name
        self.w = None
        self.r = {}
        self.semcnt = 0
        self.sem = None


class Sched:
    ENG = ("pe", "act", "dve", "pool", "sp")
    SAME_SYNC = {"pe": False, "act": True, "dve": True, "pool": True, "sp": False}

    def __init__(self, nc, st, n_dma_sems):
        self.nc = nc
        self.q = {e: [] for e in self.ENG}
        self.cnt = {e: 0 for e in self.ENG}
        self.known = {e: {} for e in self.ENG}
        self.esem = {e: st.enter_context(nc.semaphore("s_" + e)) for e in self.ENG}
        self.dpool = [st.enter_context(nc.semaphore("d%d" % i)) for i in range(n_dma_sems)]
        self.dnext = 0
        self.chans = []
        self.ninst = 0
        self.pe_isa = 0
        self.pe_sem_val = 0
        self.pe_zone_done = 0

    def _deps(self, eng, r, w):
        need = {}
        kn = self.known[eng]
        me = ("eng", eng)

        def add(ev, kind):
            k, v = ev
            if k == me and not self.SAME_SYNC[eng]:
                return
            if kn.get(k, 0) >= v:
                return
            if need.get(k, 0) < v:
                need[k] = v

        for b in r:
            if b.w is not None:
                add(b.w, "raw")
        for b in w:
            if b.w is not None:
                add(b.w, "waw")
            for k, v in b.r.items():
                add((k, v), "war")
        for k, v in need.items():
            kn[k] = v
        return list(need.items())

    def op(self, eng, fn, r=(), w=(), chan=None, kind=None):
        waits = self._deps(eng, r, w)
        if eng == "pe":
            lk = getattr(self, "_pe_kind", None)
            import os as _osq
            _ser = _osq.environ.get("PESER", "1") == "1"
            if lk is not None and (kind != lk or _ser) and self.cnt["pe"] > 0:
                k = ("eng", "pe")
                if self.known["pe"].get(k, 0) < self.cnt["pe"]:
                    self.known["pe"][k] = self.cnt["pe"]
                    waits = [wv for wv in waits if wv[0] != k] + [(k, self.cnt["pe"])]
            self._pe_kind = kind
        if chan is None:
            self.cnt[eng] += 1
            ev = (("eng", eng), self.cnt[eng])
        else:
            if chan.sem is None:
                chan.sem = self.dpool[self.dnext]
                self.dnext += 1
                self.chans.append(chan)
            chan.semcnt += 16
            ev = (("dma", chan), chan.semcnt)
        self.q[eng].append((waits, fn, ev))
        k, v = ev
        for b in r:
            if b.r.get(k, 0) < v:
                b.r[k] = v
        for b in w:
            b.w = ev
            b.r = {}
        return ev

    def dma(self, eng, out, in_, r, w, chan=None, **kw):
        ch = chan if chan is not None else w[0]
        return self.op(eng, lambda e: e.dma_start(out=out, in_=in_, **kw), r, w, chan=ch)

    def wait_events(self, eng, events):
        need = {}
        kn = self.known[eng]
        for k, v in events:
            if kn.get(k, 0) >= v:
                continue
            if need.get(k, 0) < v:
                need[k] = v
        for k, v in need.items():
            kn[k] = v
        if need:
            self.q[eng].append((list(need.items()), None, None))

    def all_events(self):
        evs = [(("eng", e), self.cnt[e]) for e in self.ENG if self.cnt[e] > 0]
        evs += [(("dma", c), c.semcnt) for c in self.chans]
        return evs

    def barrier(self):
        evs = self.all_events()
        for e in self.ENG:
            self.wait_events(e, evs)

    def _sem(self, k):
        return self.esem[k[1]] if k[0] == "eng" else k[1].sem

    def flush(self):
        nc = self.nc
        pew = set()
        for e_ in self.ENG:
            for waits, fn, ev in self.q[e_]:
                for k, v in waits:
                    if k == ("eng", "pe"):
                        pew.add(v)
        pe_ops = [ev[1] for waits, fn, ev in self.q["pe"] if fn is not None]
        if pe_ops:
            pew.add(pe_ops[-1])
        self._pew = pew
        with nc.Block() as block:
            def mk(e):
                def body(engh):
                    import os as _osp
                    for waits, fn, ev in self.q[e]:
                        for k, v in waits:
                            engh.wait_ge(self._sem(k), v)
                            self.ninst += 1
                            if e == "pe":
                                self.pe_isa += 1
                        if fn is None:
                            continue
                        if e == "pe":
                            _off = int(_osp.environ.get("PEOFF", "330"))
                            _half = int(_osp.environ.get("PEZONE", "0"))
                            _est = self.pe_isa + _off
                            _nb = (_est + _half) // 16384
                            if _nb > self.pe_zone_done:
                                self.pe_zone_done = _nb
                                for _ in range(2 * _half):
                                    engh.wait_ge(self.esem["pe"], 0)
                                self.pe_isa += 2 * _half
                            if (self.pe_isa + int(_osp.environ.get("PEPAD", "0"))) % 2 == 1:
                                engh.wait_ge(self.esem["pe"], 0)
                                self.pe_isa += 1
                            self.pe_isa += 2
                        ins = fn(engh)
                        ins.then_inc(self._sem(ev[0]), 16 if ev[0][0] == "dma" else 1)
                        self.ninst += 1
                return body

            block.tensor(mk("pe"))
            block.scalar(mk("act"))
            block.vector(mk("dve"))
            block.gpsimd(mk("pool"))
            block.sync(mk("sp"))
        if not hasattr(self, "stats"):
            self.stats = []
        self.stats.append({e: (len(self.q[e]), sum(len(w) for w, _, _ in self.q[e])) for e in self.ENG})
        self.q = {e: [] for e in self.ENG}


PARAM_SHAPES = [
    ("norm_mix_g", [D]), ("w_in", [D, NCOL]), ("rwkv_mu", [3360]), ("rwkv_w0", [1024]),
    ("rwkv_w_up", [64, 1024]), ("rwkv_a0", [1024]), ("rwkv_a_up", [64, 1024]), ("rwkv_g_up", [160, 1024]),
    ("rwkv_k_k", [1024]), ("rwkv_k_a", [1024]), ("rwkv_r_k", [1024]), ("rwkv_ln_w", [1024]),
    ("rwkv_ln_b", [1024]), ("rwkv_w_out", [1024, D]), ("s5_lam_re", [64, 64]), ("s5_lam_im", [64, 64]),
    ("s5_log_step", [64]), ("s5_b_re", [64, 64, 16]), ("s5_b_im", [64, 64, 16]), ("s5_c_re", [64, 16, 64]),
    ("s5_c_im", [64, 16, 64]), ("s5_d", [1024]), ("s5_w_glu_v", [1024, D]), ("s5_w_glu_g", [1024, D]),
    ("w_out", [D, D]), ("norm_ffn_g", [D]), ("router_group_w", [D, 4]), ("router_group_b", [4]),
    ("router_expert_w", [D, 32]), ("router_expert_b", [32]), ("moe_w_gate", [NE, D, DE]),
    ("moe_w_up", [NE, D, DE]), ("moe_w_down", [NE, DE, D]), ("norm_final_g", [D]),
]


class _Stop(Exception):
    pass


class _SkipM(Exception):
    pass


_R = {}


def build_nc(**kw):
    try:
        return _build_nc(**kw)
    except _Stop:
        return _R["nc"], _R["dbg"], _R["S"]


def _build_nc(ntm=NTM, ntf=NTF, dbg=(), stop=None, split=None):
    nc = bass.Bass("TRN2", target_bir_lowering=False)
    P = {}
    F_ONLY = ("moe_w_gate", "moe_w_up", "moe_w_down", "router_group_w", "router_group_b", "router_expert_w",
              "router_expert_b", "norm_ffn_g", "norm_final_g")
    if split == "M":
        xin = nc.dram_tensor("x", [NB, ntm * TM, D], F32, kind="ExternalInput").ap()
    elif split is None:
        xin = nc.dram_tensor("x", [NB, SEQ, D], F32, kind="ExternalInput").ap()
    for name, shp in PARAM_SHAPES:
        if split == "M" and name in F_ONLY:
            continue
        if split == "F" and name not in F_ONLY:
            continue
        P[name] = nc.dram_tensor(name, shp, F32, kind="ExternalInput").ap()
    if split == "M":
        yout = None
        x2_ext = nc.dram_tensor("x2", [NB, ntm * TM, D], F32, kind="ExternalOutput").ap()
        st_in = nc.dram_tensor("st_in", [128, 1024], F32, kind="ExternalInput").ap()
        s5c_in = nc.dram_tensor("s5c_in", [128, 128], F32, kind="ExternalInput").ap()
        carry_in = nc.dram_tensor("carry_in", [128, 54], F32, kind="ExternalInput").ap()
        st_out = nc.dram_tensor("st_out", [128, 1024], F32, kind="ExternalOutput").ap()
        s5c_out = nc.dram_tensor("s5c_out", [128, 128], F32, kind="ExternalOutput").ap()
        carry_out = nc.dram_tensor("carry_out", [128, 54], F32, kind="ExternalOutput").ap()
    elif split == "F":
        yout = nc.dram_tensor("y", [ntf * TF, D], F32, kind="ExternalOutput").ap()
    else:
        yout = nc.dram_tensor("y", [NB * SEQ, D], F32, kind="ExternalOutput").ap()
    dbg_out = {}

    def dram(name, shape, dt):
        return nc.dram_tensor(name, shape, dt, kind="Internal").ap()

    win_bf = dram("win_bf", [D, NCOL], BF16)
    wup_bf = dram("wup_bf", [64, 1024], BF16)
    aup_bf = dram("aup_bf", [64, 1024], BF16)
    gup_bf = dram("gup_bf", [160, 1024], BF16)
    wro_bf = dram("wro_bf", [1024, D], BF16)
    wgv_bf = dram("wgv_bf", [1024, D], BF16)
    wgg_bf = dram("wgg_bf", [1024, D], BF16)
    wo_bf = dram("wo_bf", [D, D], BF16)
    wrg_bf = dram("wrg_bf", [D, 4], BF16)
    wre_bf = dram("wre_bf", [D, 32], BF16)
    wg_bf = dram("wg_bf", [NE, D, DE], BF16)
    wu_bf = dram("wu_bf", [NE, D, DE], BF16)
    wd_bf = dram("wd_bf", [NE, DE, D], BF16)
    if split == "F":
        x2_dr = nc.dram_tensor("x2in", [ntf * TF, D], F32, kind="ExternalInput").ap()
    else:
        x2_dr = dram("x2_dr", [NB * SEQ, D], F32)

    top = ExitStack()
    S = Sched(nc, top, 96)

    _R["nc"] = nc
    _R["dbg"] = dbg_out
    _R["S"] = S

    _ck = {"n": 0}

    def ckpt(name):
        if stop == name:
            import os as _os9
            _ck["n"] += 1
            if _ck["n"] <= int(_os9.environ.get("STOPIT", "0")):
                return
            S.barrier()
            S.flush()
            raise _Stop()

    def tt(eng, out, in0, in1, op, r, w):
        return S.op(eng, lambda e: e.tensor_tensor(out=out, in0=in0, in1=in1, op=op), r, w)

    def ts(eng, out, in0, s1, s2, op0, op1, r, w):
        if s2 is None:
            return S.op(eng, lambda e: e.tensor_scalar(out=out, in0=in0, scalar1=s1, scalar2=None, op0=op0), r, w)
        return S.op(eng, lambda e: e.tensor_scalar(out=out, in0=in0, scalar1=s1, scalar2=s2, op0=op0, op1=op1), r, w)

    def stt(eng, out, in0, scalar, in1, op0, op1, r, w):
        return S.op(eng, lambda e: e.scalar_tensor_tensor(out=out, in0=in0, scalar=scalar, in1=in1, op0=op0, op1=op1), r, w)

    def actf(out, in_, func, r, w, bias=0.0, scale=1.0, accum=None):
        if accum is None:
            return S.op("act", lambda e: e.activation(out=out, in_=in_, func=func, bias=bias, scale=scale), r, w)
        return S.op("act", lambda e: e.activation(out=out, in_=in_, func=func, bias=bias, scale=scale, accum_out=accum), r, w)

    def cp(eng, out, in_, r, w):
        if eng == "act":
            return actf(out, in_, AF.Identity, r, w)
        return S.op(eng, lambda e: e.tensor_copy(out=out, in_=in_), r, w)

    def mm(out, lhsT, rhs, start, stop, r, w):
        kd = ("M", int(lhsT.shape[0]), int(lhsT.shape[-1]) if len(lhsT.shape) == 2 else -1)
        return S.op("pe", lambda e: e.matmul(out, lhsT, rhs, start=start, stop=stop), r, w, kind=kd)

    def tr(out, in_, ident, r, w):
        kd = ("T", int(in_.shape[0]), int(in_.shape[-1]))
        return S.op("pe", lambda e: e.transpose(out, in_, ident), r, w, kind=kd)

    def memset(eng, ap, val, w):
        return S.op(eng, lambda e: e.memset(ap, val), (), w)

    def scan(out, d0, d1, init, r, w):
        return S.op("dve", lambda e: e.tensor_tensor_scan(out=out, data0=d0, data1=d1, initial=init, op0=ALU.mult, op1=ALU.add), r, w)

    def recip(out, in_, r, w):
        return S.op("dve", lambda e: e.reciprocal(out=out, in_=in_), r, w)

    def rsum(out, in_, r, w):
        return S.op("dve", lambda e: e.reduce_sum(out=out, in_=in_, axis=AX.X), r, w)

    def rmax(out, in_, r, w):
        return S.op("dve", lambda e: e.reduce_max(out=out, in_=in_, axis=AX.X), r, w)

    def affsel(out, in_, pattern, cmp_op, fill, base, cm, r, w):
        return S.op("pool", lambda e: e.affine_select(out=out, in_=in_, pattern=pattern, compare_op=cmp_op, fill=fill, base=base, channel_multiplier=cm), r, w)

    def dbg_dump(name, sb_ap, shape, buf, dt=F32):
        if name not in dbg:
            return
        o = nc.dram_tensor("dbg_" + name, shape, dt, kind="ExternalOutput").ap()
        dbg_out[name] = o
        S.dma("sp", o, sb_ap, r=[buf], w=[Buf("dbgo_" + name)])

    def flat128(ap):
        n = 1
        for s in ap.shape:
            n *= s
        names = " ".join("a%d" % i for i in range(len(ap.shape)))
        f = ap.rearrange("%s -> (%s)" % (names, names)) if len(ap.shape) > 1 else ap
        return f.rearrange("(p f) -> p f", p=128)

    b_mixw = Buf("mixw")
    ch_mix = Buf("ch_mix")
    mix_list = [(win_bf, "w_in"), (wup_bf, "rwkv_w_up"), (aup_bf, "rwkv_a_up"), (gup_bf, "rwkv_g_up"),
                (wro_bf, "rwkv_w_out"), (wgv_bf, "s5_w_glu_v"), (wgg_bf, "s5_w_glu_g"), (wo_bf, "w_out"),
                (wrg_bf, "router_group_w"), (wre_bf, "router_expert_w")]
    mix_list = [(d_, P[n_]) for d_, n_ in mix_list if n_ in P]
    ev = None
    for dst, src in mix_list:
        if dst is win_bf:
            d2 = flat128(dst)
            s2 = flat128(src)
            nch = 8
            fw = d2.shape[1] // nch
            for c in range(nch):
                ev = S.dma("pool", d2[:, c * fw:(c + 1) * fw], s2[:, c * fw:(c + 1) * fw], r=[], w=[], chan=ch_mix)
        else:
            ev = S.dma("pool", flat128(dst), flat128(src), r=[], w=[], chan=ch_mix)
    b_mixw.w = ev
    b_moew = []
    for g4 in range(4 if (ntf > 0 and split != "M") else 0):
        ch = Buf("ch_moe%d" % g4)
        bb = Buf("moew%d" % g4)
        for e in range(g4 * 8, g4 * 8 + 8):
            for dst, src in ((wg_bf, P["moe_w_gate"]), (wu_bf, P["moe_w_up"]), (wd_bf, P["moe_w_down"])):
                ev = S.dma("pool", flat128(dst[e]), flat128(src[e]), r=[], w=[], chan=ch)
        bb.w = ev
        b_moew.append(bb)

    S.barrier()
    ckpt("precast")
    def sb(stk, name, shape, dt=F32):
        return stk.enter_context(nc.sbuf_tensor(name, shape, dt))

    ident = sb(top, "ident", [128, 128], BF16)
    b_ident = Buf("ident")
    identf = sb(top, "identf", [128, 128], F32)
    b_identf = Buf("identf")
    memset("pool", identf[:], 0.0, [b_identf])
    affsel(identf[:], identf[:], [[-1, 128]], ALU.not_equal, 1.0, 0, 1, [b_identf], [b_identf])
    cp("dve", ident[:], identf[:], [b_identf], [b_ident])

    pbank = [top.enter_context(nc.psum_tensor("pb%d" % i, [128, 512], F32)) for i in range(8)]
    b_pb = [Buf("pb%d" % i) for i in range(8)]
    pstate = {"i": 0}

    def nbank():
        i = pstate["i"]
        pstate["i"] = (i + 1) % 8
        return pbank[i], b_pb[i]

    def colvec(stk, name, src_ap, n):
        t = sb(stk, name, [128, n], F32)
        b = Buf(name)
        S.dma("sp", t[:], src_ap.rearrange("(c p) -> p c", p=128), r=[], w=[b], allow_slow_non_contiguous=True)
        return t, b

    def bcast_rows(stk, name, src_ap, n, parts=128):
        t = sb(stk, name, [parts, n], F32)
        b = Buf(name)
        S.dma("sp", t[:], src_ap.partition_broadcast(parts), r=[], w=[b])
        return t, b

    try:
      with ExitStack() as pm:
          if split == "F":
              raise _SkipM()
          g1col, b_g1 = colvec(pm, "g1col", P["norm_mix_g"], 16)
          mucol = sb(pm, "mucol", [128, 27], F32)
          b_mu = Buf("mucol")
          memset("pool", mucol[:], 0.0, [b_mu])
          S.dma("sp", mucol[:, 0:26], P["rwkv_mu"][0:3328].rearrange("(c p) -> p c", p=128), r=[], w=[b_mu], allow_slow_non_contiguous=True)
          S.dma("sp", mucol[0:32, 26:27], P["rwkv_mu"][3328:3360].rearrange("(c p) -> p c", p=32), r=[], w=[b_mu], allow_slow_non_contiguous=True)
          ommcol = sb(pm, "ommcol", [128, 27], F32)
          b_omm = Buf("ommcol")
          ts("dve", ommcol[:], mucol[:], -1.0, 1.0, ALU.mult, ALU.add, [b_mu], [b_omm])
          w0col, b_w0 = colvec(pm, "w0col", P["rwkv_w0"], 8)
          a0col, b_a0 = colvec(pm, "a0col", P["rwkv_a0"], 8)
          kkcol, b_kk = colvec(pm, "kkcol", P["rwkv_k_k"], 8)
          kacol, b_ka = colvec(pm, "kacol", P["rwkv_k_a"], 8)
          rkcol, b_rk = colvec(pm, "rkcol", P["rwkv_r_k"], 8)
          dcol, b_dc = colvec(pm, "dcol", P["s5_d"], 8)
          omka = sb(pm, "omka", [128, 8], F32)
          b_omka = Buf("omka")
          ts("dve", omka[:], kacol[:], -1.0, 1.0, ALU.mult, ALU.add, [b_ka], [b_omka])
          lnw, b_lnw = bcast_rows(pm, "lnw", P["rwkv_ln_w"], 1024)
          lnb, b_lnb = bcast_rows(pm, "lnb", P["rwkv_ln_b"], 1024)

          mnb = sb(pm, "mnb", [64, 128], F32)
          b_mnb = Buf("mnb")
          memset("pool", mnb[:], 1.0, [b_mnb])
          affsel(mnb[:, 0:64], mnb[:, 0:64], [[1, 64]], ALU.is_gt, 0.0, 0, -1, [b_mnb], [b_mnb])
          affsel(mnb[:, 64:128], mnb[:, 64:128], [[1, 64]], ALU.is_ge, 0.0, 0, -1, [b_mnb], [b_mnb])
          mq = sb(pm, "mq", [64, 64], F32)
          b_mq = Buf("mq")
          memset("pool", mq[:], 1.0, [b_mq])
          affsel(mq[:], mq[:], [[-1, 64]], ALU.is_gt, 0.0, 0, 1, [b_mq], [b_mq])
          identb64 = ident[0:64, 0:64]
          onesblk = sb(pm, "onesblk", [128, 128], BF16)
          b_ob = Buf("onesblk")
          memset("pool", onesblk[:], 0.0, [b_ob])
          memset("pool", onesblk[0:64, 0:64], 1.0, [b_ob])
          memset("pool", onesblk[64:128, 64:128], 1.0, [b_ob])
          ind2 = sb(pm, "ind2", [128, 2], BF16)
          b_ind2 = Buf("ind2")
          memset("pool", ind2[:], 0.0, [b_ind2])
          memset("pool", ind2[0:64, 0:1], 1.0, [b_ind2])
          memset("pool", ind2[64:128, 1:2], 1.0, [b_ind2])
          scmask = sb(pm, "scmask", [128, 256], F32)
          b_scm = Buf("scmask")
          memset("pool", scmask[:], 1.0, [b_scm])
          memset("pool", scmask[:].rearrange("p (k t) -> p k t", t=64)[:, :, 0:1], 0.0, [b_scm])

          wa_up = sb(pm, "wa_up", [128, 1024], BF16)
          b_waup = Buf("wa_up")
          b_waup2 = Buf("wa_up2")
          S.dma("sp", wa_up[0:64, :], wup_bf[:, :], r=[b_mixw], w=[b_waup])
          S.dma("sp", wa_up[64:128, :], aup_bf[:, :], r=[b_mixw], w=[b_waup2])
          gup1 = sb(pm, "gup1", [128, 1024], BF16)
          b_gup1 = Buf("gup1")
          S.dma("sp", gup1[:], gup_bf[0:128, :], r=[b_mixw], w=[b_gup1])
          gup2 = sb(pm, "gup2", [32, 1024], BF16)
          b_gup2 = Buf("gup2")
          S.dma("sp", gup2[:], gup_bf[128:160, :], r=[b_mixw], w=[b_gup2])

          ckpt("consts")
          rho = sb(pm, "rho", [128, 32], F32); b_rho = Buf("rho")
          theta = sb(pm, "theta", [128, 32], F32); b_theta = Buf("theta")
          Bl = sb(pm, "Bl", [128, 32, 2, 128], BF16); b_Bl = Buf("Bl")
          memset("pool", Bl[:], 0.0, [b_Bl])
          Cl = sb(pm, "Cl", [128, 32, 2, 128], BF16); b_Cl = Buf("Cl")
          tabS = sb(pm, "tabS", [128, 32, 128], BF16); b_tabS = Buf("tabS")
          tabC = sb(pm, "tabC", [128, 32, 128], BF16); b_tabC = Buf("tabC")
          with ExitStack() as s5s:
              ones256 = sb(s5s, "ones256", [128, 256], F32)
              b_ones = Buf("ones256")
              memset("pool", ones256[:], 1.0, [b_ones])
              lre = sb(s5s, "lre", [128, 32], F32); b_lre = Buf("lre")
              lim = sb(s5s, "lim", [128, 32], F32); b_lim = Buf("lim")
              lst = sb(s5s, "lst", [128, 32], F32); b_lst = Buf("lst")
              S.dma("sp", lre[:], P["s5_lam_re"].rearrange("(t g) p -> (g p) t", g=2), r=[], w=[b_lre], allow_slow_non_contiguous=True)
              S.dma("sp", lim[:], P["s5_lam_im"].rearrange("(t g) p -> (g p) t", g=2), r=[], w=[b_lim], allow_slow_non_contiguous=True)
              ls2 = P["s5_log_step"].rearrange("(t g) -> g t", g=2)
              b_lst2 = Buf("lst2")
              S.dma("sp", lst[0:64, :], ls2[0].partition_broadcast(64), r=[], w=[b_lst], allow_slow_non_contiguous=True)
              S.dma("sp", lst[64:128, :], ls2[1].partition_broadcast(64), r=[], w=[b_lst2], allow_slow_non_contiguous=True)
              step = sb(s5s, "step", [128, 32], F32); b_step = Buf("step")
              actf(step[:], lst[:], AF.Exp, [b_lst, b_lst2], [b_step])
              tmp32 = sb(s5s, "tmp32", [128, 32], F32); b_tmp32 = Buf("tmp32")
              tt("dve", tmp32[:], lre[:], step[:], ALU.mult, [b_lre, b_step], [b_tmp32])
              actf(rho[:], tmp32[:], AF.Exp, [b_tmp32], [b_rho])
              tt("dve", theta[:], lim[:], step[:], ALU.mult, [b_lim, b_step], [b_theta])

              def sincos(out_s, b_out_s, out_c, b_out_c, arg, b_arg, shape, nm):
                  u = sb(s5s, "sc_u" + nm, shape, F32); b_u = Buf("sc_u" + nm)
                  ki = sb(s5s, "sc_k" + nm, shape, I32); b_k = Buf("sc_k" + nm)
                  kf = sb(s5s, "sc_f" + nm, shape, F32); b_kf = Buf("sc_f" + nm)
                  for which, out, b_out in ((0, out_s, b_out_s), (1, out_c, b_out_c)):
                      ts("dve", u[:], arg, 1.0 / TWO_PI, 0.25 * which, ALU.mult, ALU.add, [b_arg], [b_u])
                      cp("dve", ki[:], u[:], [b_u], [b_k])
                      cp("dve", kf[:], ki[:], [b_k], [b_kf])
                      tt("dve", u[:], u[:], kf[:], ALU.subtract, [b_u, b_kf], [b_u])
                      ts("dve", kf[:], u[:], 0.5, None, ALU.is_gt, None, [b_u], [b_kf])
                      tt("dve", u[:], u[:], kf[:], ALU.subtract, [b_u, b_kf], [b_u])
                      ts("dve", kf[:], u[:], -0.5, None, ALU.is_lt, None, [b_u], [b_kf])
                      tt("dve", u[:], u[:], kf[:], ALU.add, [b_u, b_kf], [b_u])
                      actf(out, u[:], AF.Sin, [b_u], [b_out], scale=TWO_PI)

              sth = sb(s5s, "sth", [128, 32], F32); b_sth = Buf("sth")
              cth = sb(s5s, "cth", [128, 32], F32); b_cth = Buf("cth")
              sincos(sth[:], b_sth, cth[:], b_cth, theta[:], b_theta, [128, 32], "a")
              nre = sb(s5s, "nre", [128, 32], F32); b_nre = Buf("nre")
              nim = sb(s5s, "nim", [128, 32], F32); b_nim = Buf("nim")
              tt("dve", nre[:], rho[:], cth[:], ALU.mult, [b_rho, b_cth], [b_nre])
              ts("dve", nre[:], nre[:], -1.0, None, ALU.add, None, [b_nre], [b_nre])
              tt("dve", nim[:], rho[:], sth[:], ALU.mult, [b_rho, b_sth], [b_nim])
              den = sb(s5s, "den", [128, 32], F32); b_den = Buf("den")
              t2 = sb(s5s, "t2", [128, 32], F32); b_t2 = Buf("t2")
              tt("dve", den[:], lre[:], lre[:], ALU.mult, [b_lre], [b_den])
              tt("dve", t2[:], lim[:], lim[:], ALU.mult, [b_lim], [b_t2])
              tt("dve", den[:], den[:], t2[:], ALU.add, [b_den, b_t2], [b_den])
              recip(den[:], den[:], [b_den], [b_den])
              cre = sb(s5s, "cre", [128, 32], F32); b_cre = Buf("cre")
              cim = sb(s5s, "cim", [128, 32], F32); b_cim = Buf("cim")
              tt("dve", cre[:], nre[:], lre[:], ALU.mult, [b_nre, b_lre], [b_cre])
              tt("dve", t2[:], nim[:], lim[:], ALU.mult, [b_nim, b_lim], [b_t2])
              tt("dve", cre[:], cre[:], t2[:], ALU.add, [b_cre, b_t2], [b_cre])
              tt("dve", cre[:], cre[:], den[:], ALU.mult, [b_cre, b_den], [b_cre])
              tt("dve", cim[:], nim[:], lre[:], ALU.mult, [b_nim, b_lre], [b_cim])
              tt("dve", t2[:], nre[:], lim[:], ALU.mult, [b_nre, b_lim], [b_t2])
              tt("dve", cim[:], cim[:], t2[:], ALU.subtract, [b_cim, b_t2], [b_cim])
              tt("dve", cim[:], cim[:], den[:], ALU.mult, [b_cim, b_den], [b_cim])
              bnr = sb(s5s, "bnr", [128, 32, 16], F32); b_bnr = Buf("bnr")
              bni = sb(s5s, "bni", [128, 32, 16], F32); b_bni = Buf("bni")
              S.dma("sp", bnr[:], P["s5_b_re"].rearrange("(t g) p h -> (g p) t h", g=2), r=[], w=[b_bnr], allow_slow_non_contiguous=True)
              S.dma("sp", bni[:], P["s5_b_im"].rearrange("(t g) p h -> (g p) t h", g=2), r=[], w=[b_bni], allow_slow_non_contiguous=True)
              bbr = sb(s5s, "bbr", [128, 32, 16], F32); b_bbr = Buf("bbr")
              bbi = sb(s5s, "bbi", [128, 32, 16], F32); b_bbi = Buf("bbi")
              t3 = sb(s5s, "t3", [128, 32, 16], F32); b_t3 = Buf("t3")
              crb = cre[:].unsqueeze(2).broadcast_to([128, 32, 16])
              cib = cim[:].unsqueeze(2).broadcast_to([128, 32, 16])
              tt("dve", bbr[:], bnr[:], crb, ALU.mult, [b_bnr, b_cre], [b_bbr])
              tt("dve", t3[:], bni[:], cib, ALU.mult, [b_bni, b_cim], [b_t3])
              tt("dve", bbr[:], bbr[:], t3[:], ALU.subtract, [b_bbr, b_t3], [b_bbr])
              tt("dve", bbi[:], bni[:], crb, ALU.mult, [b_bni, b_cre], [b_bbi])
              tt("dve", t3[:], bnr[:], cib, ALU.mult, [b_bnr, b_cim], [b_t3])
              tt("dve", bbi[:], bbi[:], t3[:], ALU.add, [b_bbi, b_t3], [b_bbi])
              bex = sb(s5s, "bex", [128, 2, 32, 64], BF16); b_bex = Buf("bex")
              memset("pool", bex[:], 0.0, [b_bex])
              for ri, (src, bsrc) in enumerate(((bbr, b_bbr), (bbi, b_bbi))):
                  for tq in range(4):
                      for g2 in range(2):
                          c0 = ((2 * tq + g2) % 4) * 16
                          dst = bex[g2 * 64:(g2 + 1) * 64, ri].rearrange("p (a b) c -> p a b c", b=4)[:, :, tq, c0:c0 + 16]
                          s_ = src[g2 * 64:(g2 + 1) * 64].rearrange("p (a b) c -> p a b c", b=4)[:, :, tq, :]
                          cp("dve", dst, s_, [bsrc], [b_bex])
              for t in range(32):
                  po = 64 * ((t % 4) // 2)
                  pbt, bpb = nbank()
                  pv = pbt[:].bitcast(BF16)
                  for ri in range(2):
                      tr(pv[0:64, ri * 128:(ri + 1) * 128], bex[:, ri, t, :], ident[:], [b_bex, b_ident], [bpb])
                  cp("dve", Bl[po:po + 64, t, :, :], pv[0:64, 0:256].rearrange("p (r q) -> p r q", r=2), [bpb], [b_Bl])
              cnr = sb(s5s, "cnr", [128, 8, 64], F32); b_cnr = Buf("cnr")
              cni = sb(s5s, "cni", [128, 8, 64], F32); b_cni = Buf("cni")
              S.dma("sp", cnr[:], P["s5_c_re"].rearrange("(m g) h p -> (g h) m p", g=8), r=[], w=[b_cnr])
              S.dma("sp", cni[:], P["s5_c_im"].rearrange("(m g) h p -> (g h) m p", g=8), r=[], w=[b_cni])
              gm = sb(s5s, "gm", [128, 8], F32); b_gm = Buf("gm")
              memset("pool", gm[:], 1.0, [b_gm])
              affsel(gm[:], gm[:], [[-16, 8]], ALU.is_ge, 0.0, 0, 1, [b_gm], [b_gm])
              affsel(gm[:], gm[:], [[16, 8]], ALU.is_ge, 0.0, 15, -1, [b_gm], [b_gm])
              cex_ = sb(s5s, "cexp", [128, 2, 128], BF16); b_cex = Buf("cexp")
              for t in range(32):
                  m_ = t // 4
                  for ri, (src, bsrc, sgn) in enumerate(((cnr, b_cnr, 1.0), (cni, b_cni, -1.0))):
                      for g2 in range(2):
                          q8 = (2 * t + g2) % 8
                          ts("dve", cex_[:, ri, g2 * 64:(g2 + 1) * 64], src[:, m_, :], gm[:, q8:q8 + 1], sgn, ALU.mult, ALU.mult, [bsrc, b_gm], [b_cex])
                  pbt, bpb = nbank()
                  pv = pbt[:].bitcast(BF16)
                  for ri in range(2):
                      tr(pv[:, ri * 128:(ri + 1) * 128], cex_[:, ri, :], ident[:], [b_cex, b_ident], [bpb])
                  cp("dve", Cl[:, t, :, :], pv[:, 0:256].rearrange("p (r q) -> p r q", r=2), [bpb], [b_Cl])
              tau = sb(s5s, "tau", [128, 128], F32); b_tau = Buf("tau")
              scan(tau[:], ones256[:, 0:128], ones256[:, 0:128], 0.0, [b_ones], [b_tau])
              targ = sb(s5s, "targ", [128, 32, 128], F32); b_targ = Buf("targ")
              tt("dve", targ[:], tau[:].unsqueeze(1).broadcast_to([128, 32, 128]), theta[:].unsqueeze(2).broadcast_to([128, 32, 128]), ALU.mult, [b_tau, b_theta], [b_targ])
              sincos(tabS[:], b_tabS, tabC[:], b_tabC, targ[:], b_targ, [128, 32, 128], "b")
              S.barrier()
              S.flush()
          dbg_dump("rho", rho[:], [128, 32], b_rho)
          dbg_dump("theta", theta[:], [128, 32], b_theta)
          dbg_dump("tabS", tabS[:], [128, 32, 128], b_tabS, BF16)
          dbg_dump("tabC", tabC[:], [128, 32, 128], b_tabC, BF16)
          dbg_dump("Bl", Bl[:], [128, 32, 2, 128], b_Bl, BF16)
          dbg_dump("Cl", Cl[:], [128, 32, 2, 128], b_Cl, BF16)

          ckpt("s5setup")
          ST = sb(pm, "ST", [128, 8, NB, 64], F32); b_ST = [Buf("ST%d" % m) for m in range(8)]
          STb = sb(pm, "STb", [64, 16, NB, 64], BF16); b_STb = [Buf("STb%d" % m) for m in range(8)]
          if split == "M":
              S.dma("sp", ST[:].rearrange("p a b c -> p (a b c)"), st_in, r=[], w=b_ST, chan=Buf("ch_stin"))
              for m_ in range(8):
                  cp("act", STb[:, m_ * 2, :, :], ST[0:64, m_, :, :], [b_ST[m_]], [b_STb[m_]])
                  cp("dve", STb[:, m_ * 2 + 1, :, :], ST[64:128, m_, :, :], [b_ST[m_]], [b_STb[m_]])
          else:
              memset("pool", ST[:], 0.0, b_ST)
              memset("pool", STb[:], 0.0, b_STb)
          s5c = sb(pm, "s5c", [128, 32, 2, NB], F32); b_s5c = [Buf("s5c%d" % t) for t in range(32)]
          if split == "M":
              S.dma("sp", s5c[:].rearrange("p a b c -> p (a b c)"), s5c_in, r=[], w=b_s5c, chan=Buf("ch_s5in"))
          else:
              memset("pool", s5c[:], 0.0, b_s5c)
          carry = sb(pm, "carry", [128, 27, NB], F32); b_carry = [Buf("carry%d" % j) for j in range(27)]
          if split == "M":
              S.dma("sp", carry[:].rearrange("p a b -> p (a b)"), carry_in, r=[], w=b_carry, chan=Buf("ch_cin"))
          else:
              memset("pool", carry[:], 0.0, b_carry)

          xld_ = sb(pm, "xld", [128, D], F32); xld = [xld_, xld_]; b_xld_ = Buf("xld"); b_xld = [b_xld_, b_xld_]
          xsb = [sb(pm, "xsb%d" % i, [128, D], BF16) for i in range(2)]; b_xsb = [Buf("xsb%d" % i) for i in range(2)]
          stat = sb(pm, "stat", [128, 8], F32); b_stat = [Buf("stat%d" % i) for i in range(2)]
          hT = sb(pm, "hT", [128, 16, 256], BF16); b_hT = Buf("hT")
          NWS = 2
          wsl = [sb(pm, "wsl%d" % i, [128, 16, 256], BF16) for i in range(NWS)]; b_wsl = [Buf("wsl%d" % i) for i in range(NWS)]
          wst = {"i": 0}

          def wslot():
              i = wst["i"]
              wst["i"] = (i + 1) % NWS
              return wsl[i], b_wsl[i]

          NSC = 15
          scr = [sb(pm, "scr%d" % i, [128, 256], F32) for i in range(NSC)]; b_scr = [Buf("scr%d" % i) for i in range(NSC)]
          sst = {"i": 0}

          def nscr(i=None):
              if i is None:
                  i = sst["i"]
                  sst["i"] = (i + 1) % NSC
              return scr[i], b_scr[i]

          def scr_reset():
              sst["i"] = 0

          lA = sb(pm, "lA", [128, 256], BF16); b_lA = Buf("lA")
          lA2 = sb(pm, "lA2", [128, 256], BF16); b_lA2 = Buf("lA2")
          memset("pool", lA[:], 0.0, [b_lA])
          memset("pool", lA2[:], 0.0, [b_lA2])
          lB1 = sb(pm, "lB1", [128, 256], BF16); b_lB1 = Buf("lB1")
          lB2 = sb(pm, "lB2", [32, 256], BF16); b_lB2 = Buf("lB2")
          rkv = [sb(pm, "rkv%d" % i, [128, 3, 256], F32) for i in range(2)]; b_rkv = [Buf("rkv%d" % i) for i in range(2)]
          vTb = [sb(pm, "vTb%d" % i, [128, 256], BF16) for i in range(2)]; b_vTb = [Buf("vTb%d" % i) for i in range(2)]
          ARh = sb(pm, "ARh", [64, 2, 4, 2, 64], BF16); b_ARh = Buf("ARh")
          BKh = sb(pm, "BKh", [64, 2, 2, 256], BF16); b_BKh = Buf("BKh")
          tokBK = [sb(pm, "tokBK%d" % i, [64, 4, 2, 128], BF16) for i in range(2)]; b_tokBK = [Buf("tokBK%d" % i) for i in range(2)]
          tokV = [sb(pm, "tokV%d" % i, [64, 4, 128], BF16) for i in range(2)]; b_tokV = [Buf("tokV%d" % i) for i in range(2)]
          tokV2 = [sb(pm, "tokV2%d" % i, [128, NB, 128], F32) for i in range(2)]; b_tokV2 = [Buf("tokV2%d" % i) for i in range(2)]
          gtok = [sb(pm, "gtok%d" % i, [128, NB, 128], F32) for i in range(2)]; b_gtok = [Buf("gtok%d" % i) for i in range(2)]
          bon = [sb(pm, "bon%d" % i, [128, NB, 2], F32) for i in range(2)]; b_bon = [Buf("bon%d" % i) for i in range(2)]
          cL = [sb(pm, "cL%d" % i, [128, 4], F32) for i in range(2)]; b_cL = [Buf("cL%d" % i) for i in range(2)]
          NBR = sb(pm, "NBR", [64, 8, 128], BF16); b_NBR = Buf("NBR")
          KAR = sb(pm, "KAR", [64, 8, 128], BF16); b_KAR = Buf("KAR")
          Pm = [sb(pm, "Pm%d" % i, [64, 8, 64], BF16) for i in range(2)]; b_Pm = [Buf("Pm%d" % i) for i in range(2)]
          Qm = [sb(pm, "Qm%d" % i, [64, 8, 64], BF16) for i in range(2)]; b_Qm = [Buf("Qm%d" % i) for i in range(2)]
          Am = [sb(pm, "Am%d" % i, [64, 8, 64], F32) for i in range(2)]; b_Am = [Buf("Am%d" % i) for i in range(2)]
          Amb = sb(pm, "Amb", [64, 8, 64], BF16); b_Amb = Buf("Amb")
          Xs = sb(pm, "Xs", [64, 4, 64], BF16); b_Xs = Buf("Xs")
          Us = sb(pm, "Us", [64, NB, 128], BF16); b_Us = Buf("Us")
          Ytok = sb(pm, "Ytok", [128, NB, 128], F32); b_Ytok = Buf("Ytok")
          gnt = [sb(pm, "gnt%d" % i, [128, NB, 128], F32) for i in range(2)]; b_gnt = [Buf("gnt%d" % i) for i in range(2)]
          gst = sb(pm, "gst", [128, 8], F32); b_gst = Buf("gst")
          ygtok = sb(pm, "ygtok", [128, NB, 128], BF16); b_ygtok = Buf("ygtok")
          ygT = sb(pm, "ygT", [128, 8, 256], BF16); b_ygT = Buf("ygT")
          uT = sb(pm, "uT", [128, 8, 256], BF16); b_uT = Buf("uT")
          zT = sb(pm, "zT", [128, 8, 256], BF16); b_zT = Buf("zT")
          xs5_ = sb(pm, "xs5", [128, 4, 2, 256], BF16); xs5 = [xs5_, xs5_]; b_xs5_ = Buf("xs5"); b_xs5 = [b_xs5_, b_xs5_]
          mixT = sb(pm, "mixT", [128, 16, 256], BF16); b_mixT = Buf("mixT")
          wsm = [sb(pm, "wsm%d" % i, [128, 8, 128], BF16) for i in range(4)]; b_wsm = [Buf("wsm%d" % i) for i in range(4)]
          wsmst = {"i": 0}

          def wsmslot():
              i = wsmst["i"]
              wsmst["i"] = (i + 1) % 4
              return wsm[i], b_wsm[i]

          xres = [sb(pm, "xres%d" % i, [128, 256], F32) for i in range(2)]; b_xres = [Buf("xres%d" % i) for i in range(2)]
          x2o = [sb(pm, "x2o%d" % i, [128, 256], F32) for i in range(2)]; b_x2o = [Buf("x2o%d" % i) for i in range(2)]
          b_x2st = [Buf("x2st%d" % i) for i in range(2)]
          b_x2dr = Buf("x2dr")

          print("phase M sbuf remaining", nc.sbuf_bytes_remaining, flush=True)
          win_v = win_bf.rearrange("(kc p) c -> p kc c", p=128)

          def load_w(c0, ncols):
              w_, bw = wslot()
              S.dma("sp", w_[:, :, 0:ncols], win_v[:, :, c0:c0 + ncols], r=[b_mixw], w=[bw])
              return w_, bw

          def proj(w_, bw, wc0, ncols, out_ps, bps):
              for kc in range(16):
                  mm(out_ps, w_[:, kc, wc0:wc0 + ncols], hT[:, kc, :], kc == 0, kc == 15, [bw, b_hT], [bps])

          def shift_evac(ps, bps, j, rows, out, bout, eng2="dve"):
              tmp, btmp = nscr()
              p3 = ps.rearrange("p (b t) -> p b t", b=NB)
              t3_ = tmp[0:rows, :].rearrange("p (b t) -> p b t", b=NB)
              ts("dve", t3_[:, :, 1:TM], p3[:, :, 0:TM - 1], mucol[0:rows, j:j + 1], None, ALU.mult, None, [bps, b_mu], [btmp])
              ts("dve", t3_[:, :, 0:1], carry[0:rows, j, :].unsqueeze(2), mucol[0:rows, j:j + 1], None, ALU.mult, None, [b_carry[j], b_mu], [btmp])
              cp("act", carry[0:rows, j, :].unsqueeze(2), p3[:, :, TM - 1:TM], [bps], [b_carry[j]])
              stt(eng2, out, ps, ommcol[0:rows, j:j + 1], tmp[0:rows, :], ALU.mult, ALU.add, [bps, b_omm, btmp], [bout])

          for it in range(ntm):
              t0 = it * TM
              import os as _os7
              if _os7.environ.get("T0ZERO", "") == "1":
                  t0 = 0
              pstate["i"] = 0
              for b in range(NB):
                  S.dma("sp", xld[b][:], xin[b, t0:t0 + TM, :], r=[], w=[b_xld[b]])
                  actf(xsb[b][:], xld[b][:], AF.Square, [b_xld[b]], [b_xsb[b], b_stat[b]], accum=stat[:, 4 * b:4 * b + 1])
                  actf(stat[:, 4 * b + 1:4 * b + 2], stat[:, 4 * b:4 * b + 1], AF.Sqrt, [b_stat[b]], [b_stat[b]], bias=RMS_EPS, scale=1.0 / D)
                  recip(stat[:, 4 * b + 2:4 * b + 3], stat[:, 4 * b + 1:4 * b + 2], [b_stat[b]], [b_stat[b]])
                  actf(xsb[b][:], xld[b][:], AF.Identity, [b_xld[b], b_stat[b]], [b_xsb[b]], scale=stat[:, 4 * b + 2:4 * b + 3])
                  for half in range(2):
                      pbt, bpb = nbank()
                      pv = pbt[:].bitcast(BF16)
                      for k8 in range(8):
                          kc = half * 8 + k8
                          tr(pv[:, k8 * 128:(k8 + 1) * 128], xsb[b][:, kc * 128:(kc + 1) * 128], ident[:], [b_xsb[b], b_ident], [bpb])
                      tt("dve", hT[:, half * 8:half * 8 + 8, b * TM:(b + 1) * TM], pv[:, :].rearrange("p (k t) -> p k t", k=8),
                         g1col[:, half * 8:half * 8 + 8].unsqueeze(2).broadcast_to([128, 8, TM]), ALU.mult, [bpb, b_g1], [b_hT])
              if it == 0:
                  dbg_dump("hT", hT[:], [128, 16, 256], b_hT, BF16)

              ckpt("stage1")
              scr_reset()
              w_, bw = load_w(3072, 256)
              pbt, bpb = nbank()
              proj(w_, bw, 0, 128, pbt[:, 0:256], bpb)
              tA, btA = nscr()
              shift_evac(pbt[:, 0:256], bpb, 24, 128, tA[:], btA)
              actf(lA[0:64, :], tA[0:64, :], AF.Tanh, [btA], [b_lA])
              cp("dve", lA2[64:128, :], tA[64:128, :], [btA], [b_lA2])
              pbt, bpb = nbank()
              proj(w_, bw, 128, 128, pbt[:, 0:256], bpb)
              tB, btB = nscr()
              shift_evac(pbt[:, 0:256], bpb, 25, 128, tB[:], btB)
              actf(lB1[:], tB[:], AF.Sigmoid, [btB], [b_lB1])
              w_, bw = load_w(3328, 32)
              pbt, bpb = nbank()
              proj(w_, bw, 0, 32, pbt[0:32, 0:256], bpb)
              tB2, btB2 = nscr()
              shift_evac(pbt[0:32, 0:256], bpb, 26, 32, tB2[0:32, :], btB2)
              actf(lB2[:], tB2[0:32, :], AF.Sigmoid, [btB2], [b_lB2])

              ckpt("lora")
              for m in range(8):
                  pp = m % 2
                  scr_reset()
                  R3 = rkv[pp]; bR3 = b_rkv[pp]
                  wr_ = None
                  for which, c0 in enumerate((m * 128, 1024 + m * 128, 2048 + m * 128)):
                      w_, bw = load_w(c0, 128)
                      pbt, bpb = nbank()
                      proj(w_, bw, 0, 128, pbt[:, 0:256], bpb)
                      shift_evac(pbt[:, 0:256], bpb, which * 8 + m, 128, R3[:, which, :], bR3)
                  if m == 0: ckpt("rw_proj")
                  rT = R3[:, 0, :]; kT = R3[:, 1, :]; vT = R3[:, 2, :]
                  cp("act", vTb[pp][:], vT, [bR3], [b_vTb[pp]])
                  pbt, bpb = nbank()
                  mm(pbt[:, 0:256], wa_up[:, m * 128:(m + 1) * 128], lA[:, :], True, True, [b_waup, b_waup2, b_lA], [bpb])
                  if m == 0: ckpt("rw_m1")
                  mm(pbt[:, 256:512], wa_up[:, m * 128:(m + 1) * 128], lA2[:, :], True, True, [b_waup, b_waup2, b_lA2], [bpb])
                  if m == 0: ckpt("rw_m2")
                  sgw, bsgw = nscr()
                  aic, baic = nscr()
                  actf(sgw[:], pbt[:, 0:256], AF.Sigmoid, [bpb, b_w0], [bsgw], bias=w0col[:, m:m + 1])
                  actf(aic[:], pbt[:, 256:512], AF.Sigmoid, [bpb, b_a0], [baic], bias=a0col[:, m:m + 1])
                  if m == 0: ckpt("rw_l1")
                  pbt, bpb = nbank()
                  for b in range(NB):
                      mm(pbt[:, b * 128:(b + 1) * 128], lB1[:, b * TM:(b + 1) * TM], gup1[:, m * 128:(m + 1) * 128], True, False, [b_lB1, b_gup1], [bpb])
                      mm(pbt[:, b * 128:(b + 1) * 128], lB2[:, b * TM:(b + 1) * TM], gup2[:, m * 128:(m + 1) * 128], False, True, [b_lB2, b_gup2], [bpb])
                  cp("act", gtok[pp][:], pbt[:, 0:256].rearrange("p (b f) -> p b f", b=NB), [bpb], [b_gtok[pp]])
                  if m == 0: ckpt("rw_lora")
                  kk, bkk = nscr()
                  kk2, bkk2 = nscr()
                  ts("dve", kk[:], kT, kkcol[:, m:m + 1], None, ALU.mult, None, [bR3, b_kk], [bkk])
                  kk2b = kk2[:].bitcast(BF16)[:, 0:256]
                  tt("pool", kk2b, kk[:], kk[:], ALU.mult, [bkk], [bkk2])
                  pbt, bpb = nbank()
                  mm(pbt[:, 0:256], onesblk[:], kk2b, True, True, [b_ob, bkk2], [bpb])
                  rn, brn = nscr()
                  actf(rn[:], pbt[:, 0:256], AF.Sqrt, [bpb], [brn])
                  ts("dve", rn[:], rn[:], 1e-12, None, ALU.max, None, [brn], [brn])
                  recip(rn[:], rn[:], [brn], [brn])
                  tt("dve", kk[:], kk[:], rn[:], ALU.mult, [bkk, brn], [bkk])
                  if m == 0: ckpt("rw_kk")
                  cs, bcs = nscr()
                  scan(cs[:], scmask[:], sgw[:], 0.0, [b_scm, bsgw], [bcs])
                  cin, bcin = nscr()
                  cinv, bcinv = nscr()
                  cex, bcex = nscr()
                  actf(cin[:], cs[:], AF.Exp, [bcs], [bcin], scale=-DEC_K)
                  actf(cinv[:], cs[:], AF.Exp, [bcs], [bcinv], scale=DEC_K)
                  tt("pool", cex[:], cs[:], sgw[:], ALU.subtract, [bcs, bsgw], [bcex])
                  actf(cex[:], cex[:], AF.Exp, [bcex], [bcex], scale=-DEC_K)
                  import os as _os2
                  if _os2.environ.get("DBGV", "") != "K":
                      cp("dve", cL[pp][:].unsqueeze(2), cin[:].rearrange("p (u t) -> p u t", t=64)[:, :, 63:64], [bcin], [b_cL[pp]])
                  bAR = b_ARh; bBK = b_BKh
                  t1, bt1 = nscr()
                  tt("pool", t1[:], kk[:], aic[:], ALU.mult, [bkk, baic], [bt1])
                  kmod, bkmod = nscr()
                  ts("dve", kmod[:], aic[:], kacol[:, m:m + 1], omka[:, m:m + 1], ALU.mult, ALU.add, [baic, b_ka, b_omka], [bkmod])
                  tt("dve", kmod[:], kmod[:], kT, ALU.mult, [bkmod, bR3], [bkmod])
                  v3 = lambda a: a.rearrange("p (u t) -> p u t", t=64)
                  for h2 in range(2):
                      rws = slice(h2 * 64, (h2 + 1) * 64)
                      e1 = "pool" if h2 == 0 else "dve"
                      stt("dve", ARh[:, h2, :, 0, :], v3(kk[rws, :]), -1.0, v3(cex[rws, :]), ALU.mult, ALU.mult, [bkk, bcex], [bAR])
                      tt(e1, ARh[:, h2, :, 1, :], v3(R3[rws, 0, :]), v3(cin[rws, :]), ALU.mult, [bR3, bcin], [bAR])
                      tt("dve", BKh[:, h2, 0, :], t1[rws, :], cinv[rws, :], ALU.mult, [bt1, bcinv], [bBK])
                      tt(e1, BKh[:, h2, 1, :], kmod[rws, :], cinv[rws, :], ALU.mult, [bkmod, bcinv], [bBK])
                  prod, bprod = nscr()
                  prodb = prod[:].bitcast(BF16)[:, 0:256]
                  stt("dve", prodb, rT, rkcol[:, m:m + 1], kmod[:], ALU.mult, ALU.mult, [bR3, b_rk, bkmod], [bprod])
                  pbt, bpb = nbank()
                  for b in range(NB):
                      mm(pbt[:, b * 2:b * 2 + 2], prodb[:, b * TM:(b + 1) * TM], ind2[:], True, True, [bprod, b_ind2], [bpb])
                  cp("act", bon[pp][:], pbt[:, 0:4].rearrange("p (b h) -> p b h", b=NB), [bpb], [b_bon[pp]])
                  if m == 0: ckpt("rw_bonus")
                  pbt, bpb = nbank()
                  pv = pbt[:].bitcast(BF16)
                  for u in range(4):
                      for w2 in range(2):
                          for h2 in range(2):
                              c0_ = (u * 2 + w2) * 128 + h2 * 64
                              tr(pv[0:64, c0_:c0_ + 64], BKh[:, h2, w2, u * 64:(u + 1) * 64], ident[0:64, 0:64], [bBK, b_ident], [bpb])
                  cp("dve", tokBK[pp][:], pv[0:64, :].rearrange("p (u w f) -> p u w f", u=4, w=2), [bpb], [b_tokBK[pp]])
                  pbt, bpb = nbank()
                  pv = pbt[:].bitcast(BF16)
                  for u in range(4):
                      tr(pv[0:64, u * 128:(u + 1) * 128], vTb[pp][:, u * 64:(u + 1) * 64], ident[:], [b_vTb[pp], b_ident], [bpb])
                  for b in range(NB):
                      tr(pv[:, 512 + b * 128:512 + (b + 1) * 128], vTb[pp][:, b * TM:(b + 1) * TM], ident[:], [b_vTb[pp], b_ident], [bpb])
                  cp("act", tokV[pp][:], pv[0:64, 0:512].rearrange("p (u f) -> p u f", u=4), [bpb], [b_tokV[pp]])
                  cp("act", tokV2[pp][:], pv[:, 512:768].rearrange("p (b f) -> p b f", b=NB), [bpb], [b_tokV2[pp]])

                  if m == 0 and stop == "rw_tr": memset("dve", stat[:, 7:8], 1.0, [Buf("dummyM")])
                  if m == 0: ckpt("rw_tr")
                  ps1, bps1 = nbank()
                  ps1b, bps1b = nbank()
                  ps2, bps2 = nbank()
                  ps2b, bps2b = nbank()
                  ps3, bps3 = nbank()
                  import os as _os4
                  if _os4.environ.get("SWAPB", "") == "1":
                      ps1, bps1, ps3, bps3 = ps3, bps3, ps1, bps1
                  for h2 in range(2):
                      for u in range(4):
                          u8 = h2 * 4 + u
                          arhs = ARh[:, h2, u, :, :].rearrange("p a t -> p (a t)")
                          bl = BKh[:, h2, 0, u * 64:(u + 1) * 64]
                          kl = BKh[:, h2, 1, u * 64:(u + 1) * 64]
                          o1, bo1 = (ps1, bps1) if u8 < 4 else (ps1b, bps1b)
                          o2, bo2 = (ps2, bps2) if u8 < 4 else (ps2b, bps2b)
                          import os as _os3
                          _ns = int(_os3.environ.get("NSCORE", "99"))
                          _kinds = _os3.environ.get("SKIND", "123")
                          if u8 < _ns and "1" in _kinds:
                              mm(o1[0:64, (u8 % 4) * 128:(u8 % 4 + 1) * 128], bl, arhs, True, True, [bBK, bAR], [bo1])
                          if u8 < _ns and "2" in _kinds:
                              mm(o2[0:64, (u8 % 4) * 128:(u8 % 4 + 1) * 128], kl, arhs, True, True, [bBK, bAR], [bo2])
                          if u8 < _ns and "3" in _kinds:
                              mm(ps3[0:64, u8 * 64:(u8 + 1) * 64], ARh[:, h2, u, 0, :], bl, True, True, [bBK, bAR], [bps3])
                  if m == 0 and stop == "rw_scm": memset("dve", stat[:, 7:8], 1.0, [Buf("dummyM")])
                  if m == 0: ckpt("rw_scm")
                  mnb_b = mnb[:].unsqueeze(1).broadcast_to([64, 4, 128])
                  tt("dve", NBR[:, 0:4, :], ps1[0:64, :].rearrange("p (u f) -> p u f", u=4), mnb_b, ALU.mult, [bps1, b_mnb], [b_NBR])
                  tt("dve", NBR[:, 4:8, :], ps1b[0:64, :].rearrange("p (u f) -> p u f", u=4), mnb_b, ALU.mult, [bps1b, b_mnb], [b_NBR])
                  tt("dve", KAR[:, 0:4, :], ps2[0:64, :].rearrange("p (u f) -> p u f", u=4), mnb_b, ALU.mult, [bps2, b_mnb], [b_KAR])
                  tt("dve", KAR[:, 4:8, :], ps2b[0:64, :].rearrange("p (u f) -> p u f", u=4), mnb_b, ALU.mult, [bps2b, b_mnb], [b_KAR])
                  tt("dve", Qm[0][:], ps3[0:64, :].rearrange("p (u f) -> p u f", u=8), mq[:].unsqueeze(1).broadcast_to([64, 8, 64]), ALU.mult, [bps3, b_mq], [b_Qm[0]])
                  if m == 0: ckpt("rw_sc")
                  Pcur, bPcur = NBR[:, :, 0:64], b_NBR
                  qi = 0
                  ai = 0
                  import os as _os
                  _v = _os.environ.get("DBGV", "")
                  if _v == "A":
                      tt("dve", Am[0][:], NBR[:, :, 0:64], identf[0:64, 0:64].unsqueeze(1).broadcast_to([64, 8, 64]), ALU.add, [b_NBR, b_identf], [b_Am[0]])
                  elif _v == "B":
                      memset("dve", Am[0][:], 1.0, [b_Am[0]])
                      cp("act", Amb[:], Am[0][:], [b_Am[0]], [b_Amb])
                  elif _v == "C":
                      memset("dve", Am[0][:], 1.0, [b_Am[0]])
                  elif _v == "G":
                      memset("dve", stat[:, 7:8], 1.0, [Buf("dummyG")])
                  elif _v == "G2":
                      memset("dve", stat[:, 7:8], 1.0, [Buf("dummyG")])
                      memset("dve", stat[:, 6:7], 1.0, [Buf("dummyG2")])
                  elif _v == "GP":
                      memset("pool", stat[:, 7:8], 1.0, [Buf("dummyG")])
                  elif _v == "K":
                      memset("dve", stat[:, 7:8], 1.0, [Buf("dummyG")])
                  elif _v == "H":
                      pass
                  elif _v == "D":
                      memset("dve", mixT[:], 1.0, [b_mixT])
                  elif _v == "E":
                      memset("dve", Am[1][:], 1.0, [b_Am[1]])
                  elif _v == "F":
                      memset("dve", Qm[1][:], 1.0, [b_Qm[1]])
                  else:
                      tt("dve", Am[0][:], NBR[:, :, 0:64], identf[0:64, 0:64].unsqueeze(1).broadcast_to([64, 8, 64]), ALU.add, [b_NBR, b_identf], [b_Am[0]])
                      cp("act", Amb[:], Am[0][:], [b_Am[0]], [b_Amb])
                  if m == 0: ckpt("rw_c0")
                  for lvl in range(1, 6):
                      Qc, bQc = Qm[qi], b_Qm[qi]
                      Qn, bQn = Qm[1 - qi], b_Qm[1 - qi]
                      psq, bpsq = nbank()
                      for u8 in range(8):
                          mm(psq[0:64, u8 * 64:(u8 + 1) * 64], Pcur[:, u8, :], Qc[:, u8, :], True, True, [bPcur, bQc], [bpsq])
                      if lvl < 5:
                          psp, bpsp = nbank()
                          for u8 in range(8):
                              mm(psp[0:64, u8 * 64:(u8 + 1) * 64], Qc[:, u8, :], Pcur[:, u8, :], True, True, [bPcur, bQc], [bpsp])
                      cp("act", Qn[:], psq[0:64, :].rearrange("p (u f) -> p u f", u=8), [bpsq], [bQn])
                      if lvl < 5:
                          Pn, bPn = Pm[lvl % 2], b_Pm[lvl % 2]
                          cp("dve", Pn[:], psp[0:64, :].rearrange("p (u f) -> p u f", u=8), [bpsp], [bPn])
                          Pcur, bPcur = Pn[:], bPn
                      qi = 1 - qi
                      psa, bpsa = nbank()
                      for u8 in range(8):
                          mm(psa[0:64, u8 * 64:(u8 + 1) * 64], Qn[:, u8, :], Amb[:, u8, :], True, True, [bQn, b_Amb], [bpsa])
                      tt("dve", Am[1 - ai][:], psa[0:64, :].rearrange("p (u f) -> p u f", u=8), Am[ai][:], ALU.add, [bpsa, b_Am[ai]], [b_Am[1 - ai]])
                      ai = 1 - ai
                      cp("act", Amb[:], Am[ai][:], [b_Am[ai]], [b_Amb])
                      if m == 0 and lvl == 1: ckpt("rw_c1")
                  if m == 0: ckpt("rw_chain")
                  for c in range(2):
                      psx, bpsx = nbank()
                      for b in range(NB):
                          for h2 in range(2):
                              rows = slice(h2 * 64, (h2 + 1) * 64)
                              u = b * 2 + c
                              u8 = h2 * 4 + u
                              o = psx[0:64, (b * 2 + h2) * 64:(b * 2 + h2 + 1) * 64]
                              mm(o, ARh[:, h2, u, 0, :], STb[:, m * 2 + h2, b, :], True, False, [bAR, b_STb[m]], [bpsx])
                              mm(o, KAR[:, u8, 0:64], tokV[pp][:, u, h2 * 64:(h2 + 1) * 64], False, True, [b_KAR, b_tokV[pp]], [bpsx])
                      cp("act", Xs[:], psx[0:64, 0:256].rearrange("p (u f) -> p u f", u=4), [bpsx], [b_Xs])
                      psu, bpsu = nbank()
                      for b in range(NB):
                          for h2 in range(2):
                              u8 = h2 * 4 + b * 2 + c
                              mm(psu[0:64, (b * 2 + h2) * 64:(b * 2 + h2 + 1) * 64], Amb[:, u8, :], Xs[:, b * 2 + h2, :], True, True, [b_Amb, b_Xs], [bpsu])
                      cp("dve", Us[:], psu[0:64, 0:256].rearrange("p (b f) -> p b f", b=NB), [bpsu], [b_Us])
                      psy, bpsy = nbank()
                      for b in range(NB):
                          for h2 in range(2):
                              rows = slice(h2 * 64, (h2 + 1) * 64)
                              u = b * 2 + c
                              u8 = h2 * 4 + u
                              o = psy[0:64, (b * 2 + h2) * 64:(b * 2 + h2 + 1) * 64]
                              mm(o, ARh[:, h2, u, 1, :], STb[:, m * 2 + h2, b, :], True, False, [bAR, b_STb[m]], [bpsy])
                              mm(o, NBR[:, u8, 64:128], Us[:, b, h2 * 64:(h2 + 1) * 64], False, False, [b_NBR, b_Us], [bpsy])
                              mm(o, KAR[:, u8, 64:128], tokV[pp][:, u, h2 * 64:(h2 + 1) * 64], False, True, [b_KAR, b_tokV[pp]], [bpsy])
                      cp("act", Ytok[c * 64:(c + 1) * 64, :, :], psy[0:64, 0:256].rearrange("p (b f) -> p b f", b=NB), [bpsy], [b_Ytok])
                      pss, bpss = nbank()
                      for b in range(NB):
                          u = b * 2 + c
                          o = pss[:, b * 128:(b + 1) * 128]
                          mm(o, tokBK[pp][:, u, 0, :], Us[:, b, :], True, False, [b_tokBK[pp], b_Us], [bpss])
                          mm(o, tokBK[pp][:, u, 1, :], tokV[pp][:, u, :], False, True, [b_tokBK[pp], b_tokV[pp]], [bpss])
                      for b in range(NB):
                          u = b * 2 + c
                          for h2 in range(2):
                              rows = slice(h2 * 64, (h2 + 1) * 64)
                              tt("dve", ST[rows, m, b, :], pss[rows, b * 128 + h2 * 64:b * 128 + (h2 + 1) * 64], ST[rows, m, b, :], ALU.add, [bpss, b_ST[m]], [b_ST[m]])
                          ts("dve", ST[:, m, b, :], ST[:, m, b, :], cL[pp][:, u:u + 1], None, ALU.mult, None, [b_ST[m], b_cL[pp]], [b_ST[m]])
                      cp("act", STb[:, m * 2, :, :], ST[0:64, m, :, :], [b_ST[m]], [b_STb[m]])
                      cp("dve", STb[:, m * 2 + 1, :, :], ST[64:128, m, :, :], [b_ST[m]], [b_STb[m]])
                  if it == 0 and m == 0:
                      dbg_dump("Ytok", Ytok[:], [128, NB, 128], b_Ytok)
                  if m == 0: ckpt("rw_seq")
                  Y4 = Ytok[:].rearrange("p b (h i) -> p (b h) i", h=2)
                  G0 = gnt[0][:].rearrange("p b (h i) -> p (b h) i", h=2)
                  G1 = gnt[1][:].rearrange("p b (h i) -> p (b h) i", h=2)
                  rsum(gst[:, 0:4], Y4, [b_Ytok], [b_gst])
                  ts("dve", gst[:, 0:4], gst[:, 0:4], -1.0 / 64, None, ALU.mult, None, [b_gst], [b_gst])
                  tt("dve", G0, Y4, gst[:, 0:4].unsqueeze(2).broadcast_to([128, 4, 64]), ALU.add, [b_Ytok, b_gst], [b_gnt[0]])
                  tt("pool", G1, G0, G0, ALU.mult, [b_gnt[0]], [b_gnt[1]])
                  rsum(gst[:, 4:8], G1, [b_gnt[1]], [b_gst])
                  actf(gst[:, 4:8], gst[:, 4:8], AF.Sqrt, [b_gst], [b_gst], bias=GN_EPS, scale=1.0 / 64)
                  recip(gst[:, 4:8], gst[:, 4:8], [b_gst], [b_gst])
                  tt("dve", G0, G0, gst[:, 4:8].unsqueeze(2).broadcast_to([128, 4, 64]), ALU.mult, [b_gnt[0], b_gst], [b_gnt[0]])
                  lw = lnw[:, m * 128:(m + 1) * 128].unsqueeze(1).broadcast_to([128, NB, 128])
                  lb = lnb[:, m * 128:(m + 1) * 128].unsqueeze(1).broadcast_to([128, NB, 128])
                  tt("dve", gnt[0][:], gnt[0][:], lw, ALU.mult, [b_gnt[0], b_lnw], [b_gnt[0]])
                  tt("pool", gnt[0][:], gnt[0][:], lb, ALU.add, [b_gnt[0], b_lnb], [b_gnt[0]])
                  tt("dve", G1, tokV2[pp][:].rearrange("p b (h i) -> p (b h) i", h=2),
                     bon[pp][:].rearrange("p b h -> p (b h)").unsqueeze(2).broadcast_to([128, 4, 64]), ALU.mult, [b_tokV2[pp], b_bon[pp]], [b_gnt[1]])
                  tt("pool", gnt[0][:], gnt[0][:], gnt[1][:], ALU.add, [b_gnt[0], b_gnt[1]], [b_gnt[0]])
                  tt("dve", ygtok[:], gnt[0][:], gtok[pp][:], ALU.mult, [b_gnt[0], b_gtok[pp]], [b_ygtok])
                  pbt, bpb = nbank()
                  pv = pbt[:].bitcast(BF16)
                  for b in range(NB):
                      tr(pv[:, b * 128:(b + 1) * 128], ygtok[:, b, :], ident[:], [b_ygtok, b_ident], [bpb])
                  cp("act", ygT[:, m, :], pv[:, 0:256], [bpb], [b_ygT])
              if it == 0:
                  dbg_dump("ygT", ygT[:], [128, 8, 256], b_ygT, BF16)

              ckpt("rwkv")
              w_u = [None, None, None, None]
              for j in range(4):
                  w_u[j] = load_w(3360 + j * 256, 256)
                  for jj in range(2):
                      mb = j * 2 + jj
                      pbt, bpb = nbank()
                      proj(w_u[j][0], w_u[j][1], jj * 128, 128, pbt[:, 0:256], bpb)
                      cp("dve", uT[:, mb, :], pbt[:, 0:256], [bpb], [b_uT])
              for m8 in range(8):
                  X = xs5[m8 % 2]; bX = b_xs5[m8 % 2]
                  for tq in range(4):
                      t = m8 * 4 + tq
                      po = 64 * (tq // 2)
                      scr_reset()
                      pbt, bpb = nbank()
                      for ri in range(2):
                          mm(pbt[:, ri * 256:(ri + 1) * 256], Bl[:, t, ri, :], uT[:, m8, :], True, True, [b_Bl, b_uT], [bpb])
                      tC = tabC[:, t, :].unsqueeze(1).broadcast_to([128, 4, 128])
                      tS = tabS[:, t, :].unsqueeze(1).broadcast_to([128, 4, 128])
                      a1, ba1 = nscr(); a2, ba2 = nscr()
                      pc4 = pbt[:, :].rearrange("p (q t) -> p q t", t=128)
                      a3, ba3 = nscr(); a4, ba4 = nscr()
                      tt("dve", a1[:].rearrange("p (q t) -> p q t", t=128), pc4[:, 0:2, :], tC[:, 0:2, :], ALU.mult, [bpb, b_tabC], [ba1])
                      tt("dve", a2[:].rearrange("p (q t) -> p q t", t=128), pc4[:, 2:4, :], tS[:, 0:2, :], ALU.mult, [bpb, b_tabS], [ba2])
                      tt("dve", a3[:].rearrange("p (q t) -> p q t", t=128), pc4[:, 2:4, :], tC[:, 0:2, :], ALU.mult, [bpb, b_tabC], [ba3])
                      tt("dve", a4[:].rearrange("p (q t) -> p q t", t=128), pc4[:, 0:2, :], tS[:, 0:2, :], ALU.mult, [bpb, b_tabS], [ba4])
                      tt("pool", a1[:], a1[:], a2[:], ALU.add, [ba1, ba2], [ba1])
                      tt("pool", a3[:], a3[:], a4[:], ALU.subtract, [ba3, ba4], [ba3])
                      for b in range(NB):
                          scan(a2[:, b * TM:(b + 1) * TM], rho[:, t:t + 1].broadcast_to([128, TM]), a1[:, b * TM:(b + 1) * TM], s5c[:, t, 0, b:b + 1], [b_rho, ba1, b_s5c[t]], [ba2])
                          scan(a4[:, b * TM:(b + 1) * TM], rho[:, t:t + 1].broadcast_to([128, TM]), a3[:, b * TM:(b + 1) * TM], s5c[:, t, 1, b:b + 1], [b_rho, ba3, b_s5c[t]], [ba4])
                      w_re3 = a2[:].rearrange("p (q t) -> p q t", t=128)
                      w_im3 = a4[:].rearrange("p (q t) -> p q t", t=128)
                      tt("dve", a1[:].rearrange("p (q t) -> p q t", t=128), w_re3, tC[:, 0:2, :], ALU.mult, [ba2, b_tabC], [ba1])
                      tt("pool", a3[:].rearrange("p (q t) -> p q t", t=128), w_im3, tS[:, 0:2, :], ALU.mult, [ba4, b_tabS], [ba3])
                      a5, ba5 = nscr(); a6, ba6 = nscr()
                      tt("dve", a5[:].rearrange("p (q t) -> p q t", t=128), w_re3, tS[:, 0:2, :], ALU.mult, [ba2, b_tabS], [ba5])
                      tt("pool", a6[:].rearrange("p (q t) -> p q t", t=128), w_im3, tC[:, 0:2, :], ALU.mult, [ba4, b_tabC], [ba6])
                      tt("dve", X[:, tq, 0, :], a1[:], a3[:], ALU.subtract, [ba1, ba3], [bX])
                      tt("pool", X[:, tq, 1, :], a5[:], a6[:], ALU.add, [ba5, ba6], [bX])
                      l1 = a1[:].rearrange("p (b t) -> p b t", b=NB)[:, :, TM - 1]
                      l3 = a3[:].rearrange("p (b t) -> p b t", b=NB)[:, :, TM - 1]
                      l5 = a5[:].rearrange("p (b t) -> p b t", b=NB)[:, :, TM - 1]
                      l6 = a6[:].rearrange("p (b t) -> p b t", b=NB)[:, :, TM - 1]
                      tt("dve", s5c[:, t, 0, :], l1, l3, ALU.subtract, [ba1, ba3], [b_s5c[t]])
                      tt("dve", s5c[:, t, 1, :], l5, l6, ALU.add, [ba5, ba6], [b_s5c[t]])
                  pbt, bpb = nbank()
                  for tq in range(4):
                      t = m8 * 4 + tq
                      for ri in range(2):
                          mm(pbt[:, 0:256], Cl[:, t, ri, :], X[:, tq, ri, :], tq == 0 and ri == 0, tq == 3 and ri == 1, [b_Cl, bX], [bpb])
                  yy, byy = nscr(10)
                  y2, by2 = nscr(11)
                  stt("dve", yy[:], uT[:, m8, :], dcol[:, m8:m8 + 1], pbt[:, 0:256], ALU.mult, ALU.add, [b_uT, b_dc, bpb], [byy])
                  tt("pool", y2[:], yy[:], yy[:], ALU.mult, [byy], [by2])
                  ts("dve", y2[:], y2[:], 0.044715, 1.0, ALU.mult, ALU.add, [by2], [by2])
                  tt("pool", y2[:], y2[:], yy[:], ALU.mult, [by2, byy], [by2])
                  actf(y2[:], y2[:], AF.Sigmoid, [by2], [by2], scale=1.5957691216057308)
                  tt("dve", zT[:, m8, :], y2[:], yy[:], ALU.mult, [by2, byy], [b_zT])
              if it == 0:
                  dbg_dump("zT", zT[:], [128, 8, 256], b_zT, BF16)

              ckpt("s5")
              wro_v = wro_bf.rearrange("(kc p) c -> p kc c", p=128)
              wgv_v = wgv_bf.rearrange("(kc p) c -> p kc c", p=128)
              wgg_v = wgg_bf.rearrange("(kc p) c -> p kc c", p=128)
              for f2 in range(8):
                  wga = load_w(4384 + f2 * 256, 256)
                  wgb = load_w(6432 + f2 * 256, 256)
                  for ff in range(2):
                      f = f2 * 2 + ff
                      wr1, bwr1 = wsmslot()
                      S.dma("sp", wr1[:], wro_v[:, :, f * 128:(f + 1) * 128], r=[b_mixw], w=[bwr1])
                      wr2, bwr2 = wsmslot()
                      S.dma("sp", wr2[:], wgv_v[:, :, f * 128:(f + 1) * 128], r=[b_mixw], w=[bwr2])
                      wr3, bwr3 = wsmslot()
                      S.dma("sp", wr3[:], wgg_v[:, :, f * 128:(f + 1) * 128], r=[b_mixw], w=[bwr3])
                      scr_reset()
                      pg, bpg = nbank()
                      proj(wga[0], wga[1], ff * 128, 128, pg[:, 0:256], bpg)
                      proj(wgb[0], wgb[1], ff * 128, 128, pg[:, 256:512], bpg)
                      py, bpy = nbank()
                      for kc in range(8):
                          mm(py[:, 0:256], wr1[:, kc, :], ygT[:, kc, :], kc == 0, kc == 7, [bwr1, b_ygT], [bpy])
                      for kc in range(8):
                          mm(py[:, 256:512], wr2[:, kc, :], zT[:, kc, :], kc == 0, kc == 7, [bwr2, b_zT], [bpy])
                      pq, bpq = nbank()
                      for kc in range(8):
                          mm(pq[:, 0:256], wr3[:, kc, :], zT[:, kc, :], kc == 0, kc == 7, [bwr3, b_zT], [bpq])
                      sa, bsa = nscr(); sb_, bsb_ = nscr(); sg_, bsg_ = nscr()
                      actf(sa[:], pg[:, 0:256], AF.Sigmoid, [bpg], [bsa])
                      actf(sb_[:], pg[:, 256:512], AF.Sigmoid, [bpg], [bsb_])
                      actf(sg_[:], pq[:, 0:256], AF.Sigmoid, [bpq], [bsg_])
                      tt("dve", sa[:], py[:, 0:256], sa[:], ALU.mult, [bpy, bsa], [bsa])
                      tt("dve", sg_[:], py[:, 256:512], sg_[:], ALU.mult, [bpy, bsg_], [bsg_])
                      tt("pool", sg_[:], sg_[:], sb_[:], ALU.mult, [bsg_, bsb_], [bsg_])
                      tt("pool", mixT[:, f, :], sa[:], sg_[:], ALU.add, [bsa, bsg_], [b_mixT])
              if it == 0:
                  dbg_dump("mixT", mixT[:], [128, 16, 256], b_mixT, BF16)

              ckpt("merge")
              wo_v = wo_bf.rearrange("(kc p) c -> p kc c", p=128)
              for cb in range(8):
                  w_, bw = wslot()
                  S.dma("sp", w_[:], wo_v[:, :, cb * 256:(cb + 1) * 256], r=[b_mixw], w=[bw])
                  for b in range(NB):
                      i2 = (cb * NB + b) % 2
                      S.dma("sp", xres[i2][:], xin[b, t0:t0 + TM, cb * 256:(cb + 1) * 256], r=[], w=[b_xres[i2]])
                      pbt, bpb = nbank()
                      for kc in range(16):
                          mm(pbt[:, 0:256], mixT[:, kc, b * TM:(b + 1) * TM], w_[:, kc, :], kc == 0, kc == 15, [b_mixT, bw], [bpb])
                      tt("dve", x2o[i2][:], pbt[:, 0:256], xres[i2][:], ALU.add, [bpb, b_xres[i2]], [b_x2o[i2]])
                      S.dma("sp", (x2_ext[b, t0:t0 + TM, cb * 256:(cb + 1) * 256] if split == "M" else x2_dr[b * SEQ + t0:b * SEQ + t0 + TM, cb * 256:(cb + 1) * 256]), x2o[i2][:], r=[b_x2o[i2]], w=[], chan=b_x2st[i2])
          if split == "M":
              S.dma("sp", st_out, ST[:].rearrange("p a b c -> p (a b c)"), r=b_ST, w=[], chan=Buf("ch_sto"))
              S.dma("sp", s5c_out, s5c[:].rearrange("p a b c -> p (a b c)"), r=b_s5c, w=[], chan=Buf("ch_s5o"))
              S.dma("sp", carry_out, carry[:].rearrange("p a b -> p (a b)"), r=b_carry, w=[], chan=Buf("ch_co"))
          ckpt("wout")
          S.barrier()
          S.flush()

    except _SkipM:
        pass
    except _Stop:
        raise
    if split == "M":
        top.close()
        return nc, dbg_out, S
    with ExitStack() as pf:
        g2col, b_g2 = colvec(pf, "g2col", P["norm_ffn_g"], 16)
        gF, b_gF = bcast_rows(pf, "gF", P["norm_final_g"], D)
        rbias = sb(pf, "rbias", [128, 36], F32); b_rbias = Buf("rbias")
        b_rbias2 = Buf("rbias2")
        S.dma("sp", rbias[:, 0:4], P["router_group_b"].partition_broadcast(128), r=[], w=[b_rbias])
        S.dma("sp", rbias[:, 4:36], P["router_expert_b"].partition_broadcast(128), r=[], w=[b_rbias2])
        wr = sb(pf, "wr", [128, 16, 36], BF16); b_wr = Buf("wr"); b_wr2 = Buf("wr2")
        S.dma("sp", wr[:, :, 0:4], wrg_bf.rearrange("(kc p) c -> p kc c", p=128), r=[b_mixw], w=[b_wr], allow_slow_non_contiguous=True)
        S.dma("sp", wr[:, :, 4:36], wre_bf.rearrange("(kc p) c -> p kc c", p=128), r=[b_mixw], w=[b_wr2], allow_slow_non_contiguous=True)
        acc = sb(pf, "acc", [128, 4, D], F32); b_acc = [Buf("acc%d" % q) for q in range(4)]
        xsf = sb(pf, "xsf", [128, D], BF16); b_xsf = Buf("xsf")
        h2T = sb(pf, "h2T", [128, 16, TF], BF16); b_h2T = Buf("h2T")
        fst = sb(pf, "fst", [128, 4, 4], F32); b_fst = [Buf("fst%d" % q) for q in range(4)]
        gates = sb(pf, "gates", [128, 4, NE], F32); b_gates = [Buf("gates%d" % q) for q in range(4)]
        rt = [sb(pf, "rt%d" % i, [128, 36], F32) for i in range(6)]; b_rt = [Buf("rt%d" % i) for i in range(6)]
        rs = sb(pf, "rs", [128, 8], F32); b_rs = Buf("rs")
        Wg = [sb(pf, "Wg%d" % i, [128, 16, DE], BF16) for i in range(2)]; b_Wg = [Buf("Wg%d" % i) for i in range(2)]
        Wu = [sb(pf, "Wu%d" % i, [128, 16, DE], BF16) for i in range(2)]; b_Wu = [Buf("Wu%d" % i) for i in range(2)]
        Wd = [sb(pf, "Wd%d" % i, [128, 4, D], BF16) for i in range(2)]; b_Wd = [Buf("Wd%d" % i) for i in range(2)]
        sgt = [sb(pf, "sgt%d" % i, [128, TF], F32) for i in range(2)]; b_sgt = [Buf("sgt%d" % i) for i in range(2)]
        hid = [sb(pf, "hid%d" % i, [128, 4, TF], BF16) for i in range(2)]; b_hid = [Buf("hid%d" % i) for i in range(2)]
        b_yst = [Buf("yst%d" % q) for q in range(4)]
        b_ydr = Buf("ydr")
        wg_v = wg_bf.rearrange("e (kc p) f -> e p kc f", p=128)
        wu_v = wu_bf.rearrange("e (kc p) f -> e p kc f", p=128)
        wd_v = wd_bf.rearrange("e (fc p) c -> e p fc c", p=128)

        for jt in range(ntf):
            r0 = jt * TF
            pstate["i"] = 0
            for q in range(4):
                S.dma("sp", acc[:, q, :], x2_dr[r0 + q * 128:r0 + (q + 1) * 128, :], r=[], w=[b_acc[q]])
                actf(xsf[:], acc[:, q, :], AF.Square, [b_acc[q]], [b_xsf, b_fst[q]], accum=fst[:, q, 0:1])
                actf(fst[:, q, 1:2], fst[:, q, 0:1], AF.Sqrt, [b_fst[q]], [b_fst[q]], bias=RMS_EPS, scale=1.0 / D)
                recip(fst[:, q, 2:3], fst[:, q, 1:2], [b_fst[q]], [b_fst[q]])
                actf(xsf[:], acc[:, q, :], AF.Identity, [b_acc[q], b_fst[q]], [b_xsf], scale=fst[:, q, 2:3])
                for half in range(2):
                    pbt, bpb = nbank()
                    pv = pbt[:].bitcast(BF16)
                    for k8 in range(8):
                        kc = half * 8 + k8
                        tr(pv[:, k8 * 128:(k8 + 1) * 128], xsf[:, kc * 128:(kc + 1) * 128], ident[:], [b_xsf, b_ident], [bpb])
                    tt("dve", h2T[:, half * 8:half * 8 + 8, q * 128:(q + 1) * 128], pv[:, :].rearrange("p (k t) -> p k t", k=8),
                       g2col[:, half * 8:half * 8 + 8].unsqueeze(2).broadcast_to([128, 8, 128]), ALU.mult, [bpb, b_g2], [b_h2T])
                pbt, bpb = nbank()
                for kc in range(16):
                    mm(pbt[:, 0:36], h2T[:, kc, q * 128:(q + 1) * 128], wr[:, kc, :], kc == 0, kc == 15, [b_h2T, b_wr, b_wr2], [bpb])
                lg, blg = rt[0], b_rt[0]
                tt("dve", lg[:], pbt[:, 0:36], rbias[:], ALU.add, [bpb, b_rbias, b_rbias2], [blg])
                rmax(rs[:, 0:1], lg[:, 0:4], [blg], [b_rs])
                oh, boh = rt[1], b_rt[1]
                ts("dve", oh[:, 0:4], lg[:, 0:4], rs[:, 0:1], None, ALU.is_ge, None, [blg, b_rs], [boh])
                ex, bex_ = rt[2], b_rt[2]
                ts("dve", ex[:, 0:4], lg[:, 0:4], rs[:, 0:1], None, ALU.subtract, None, [blg, b_rs], [bex_])
                actf(ex[:, 0:4], ex[:, 0:4], AF.Exp, [bex_], [bex_])
                rsum(rs[:, 1:2], ex[:, 0:4], [bex_], [b_rs])
                recip(rs[:, 2:3], rs[:, 1:2], [b_rs], [b_rs])
                msk, bmsk = rt[3], b_rt[3]
                ts("dve", oh[:, 4:8], oh[:, 0:4], -1.0, 1e30, ALU.add, ALU.mult, [boh], [boh])
                tt("dve", msk[:, 0:32].rearrange("p (g e) -> p g e", g=4), lg[:, 4:36].rearrange("p (g e) -> p g e", g=4),
                   oh[:, 4:8].unsqueeze(2).broadcast_to([128, 4, 8]), ALU.add, [blg, boh], [bmsk])
                rmax(rs[:, 3:4], msk[:, 0:32], [bmsk], [b_rs])
                m1, bm1 = rt[4], b_rt[4]
                ts("dve", m1[:, 0:32], msk[:, 0:32], rs[:, 3:4], None, ALU.is_ge, None, [bmsk, b_rs], [bm1])
                stt("dve", msk[:, 0:32], m1[:, 0:32], -1e30, msk[:, 0:32], ALU.mult, ALU.add, [bm1, bmsk], [bmsk])
                rmax(rs[:, 4:5], msk[:, 0:32], [bmsk], [b_rs])
                m2, bm2 = rt[5], b_rt[5]
                ts("dve", m2[:, 0:32], msk[:, 0:32], rs[:, 4:5], None, ALU.is_ge, None, [bmsk, b_rs], [bm2])
                tt("dve", rs[:, 5:6], rs[:, 4:5], rs[:, 3:4], ALU.subtract, [b_rs], [b_rs])
                actf(rs[:, 5:6], rs[:, 5:6], AF.Exp, [b_rs], [b_rs])
                ts("dve", rs[:, 5:6], rs[:, 5:6], 1.0, None, ALU.add, None, [b_rs], [b_rs])
                recip(rs[:, 5:6], rs[:, 5:6], [b_rs], [b_rs])
                ts("dve", rs[:, 6:7], rs[:, 5:6], -1.0, 1.0, ALU.mult, ALU.add, [b_rs], [b_rs])
                tt("dve", rs[:, 5:6], rs[:, 5:6], rs[:, 2:3], ALU.mult, [b_rs], [b_rs])
                tt("dve", rs[:, 6:7], rs[:, 6:7], rs[:, 2:3], ALU.mult, [b_rs], [b_rs])
                ts("dve", m1[:, 0:32], m1[:, 0:32], rs[:, 5:6], None, ALU.mult, None, [bm1, b_rs], [bm1])
                stt("dve", gates[:, q, :], m2[:, 0:32], rs[:, 6:7], m1[:, 0:32], ALU.mult, ALU.add, [bm2, b_rs, bm1], [b_gates[q]])
            if jt == 0:
                dbg_dump("gates", gates[:], [128, 4, NE], b_gates[3])
            for e in range(NE):
                i2 = e % 2
                bmw = b_moew[e // 8]
                S.dma("sp", Wg[i2][:], wg_v[e], r=[bmw], w=[b_Wg[i2]])
                S.dma("sp", Wu[i2][:], wu_v[e], r=[bmw], w=[b_Wu[i2]])
                S.dma("sp", Wd[i2][:], wd_v[e], r=[bmw], w=[b_Wd[i2]])
                H = hid[i2]; bH = b_hid[i2]
                for fb in range(4):
                    pg, bpg = nbank()
                    for kc in range(16):
                        mm(pg[:, :], Wg[i2][:, kc, fb * 128:(fb + 1) * 128], h2T[:, kc, :], kc == 0, kc == 15, [b_Wg[i2], b_h2T], [bpg])
                    pu, bpu = nbank()
                    for kc in range(16):
                        mm(pu[:, :], Wu[i2][:, kc, fb * 128:(fb + 1) * 128], h2T[:, kc, :], kc == 0, kc == 15, [b_Wu[i2], b_h2T], [bpu])
                    sg2 = sgt[fb % 2]; bsg2 = b_sgt[fb % 2]
                    actf(sg2[:], pg[:, :], AF.Silu, [bpg], [bsg2])
                    tt("dve", H[:, fb, :], pu[:, :], sg2[:], ALU.mult, [bpu, bsg2], [bH])
                for q in range(4):
                    for cb in range(4):
                        pd, bpd = nbank()
                        for fc in range(4):
                            mm(pd[:, :], H[:, fc, q * 128:(q + 1) * 128], Wd[i2][:, fc, cb * 512:(cb + 1) * 512], fc == 0, fc == 3, [bH, b_Wd[i2]], [bpd])
                        stt("dve", acc[:, q, cb * 512:(cb + 1) * 512], pd[:, :], gates[:, q, e:e + 1], acc[:, q, cb * 512:(cb + 1) * 512],
                            ALU.mult, ALU.add, [bpd, b_gates[q], b_acc[q]], [b_acc[q]])
            for q in range(4):
                actf(xsf[:], acc[:, q, :], AF.Square, [b_acc[q]], [b_xsf, b_fst[q]], accum=fst[:, q, 0:1])
                actf(fst[:, q, 1:2], fst[:, q, 0:1], AF.Sqrt, [b_fst[q]], [b_fst[q]], bias=RMS_EPS, scale=1.0 / D)
                recip(fst[:, q, 2:3], fst[:, q, 1:2], [b_fst[q]], [b_fst[q]])
                stt("dve", acc[:, q, :], acc[:, q, :], fst[:, q, 2:3], gF[:], ALU.mult, ALU.mult, [b_acc[q], b_fst[q], b_gF], [b_acc[q]])
                S.dma("sp", yout[r0 + q * 128:r0 + (q + 1) * 128, :], acc[:, q, :], r=[b_acc[q]], w=[], chan=b_yst[q])
        S.wait_events("sp", S.all_events())
        S.flush()
    top.close()
    return nc, dbg_out, S


_CACHE = {}


def kernel(**inputs):
    x = np.ascontiguousarray(np.asarray(inputs["x"], dtype=np.float32))
    if "nc" not in _CACHE:
        _CACHE["nc"] = build_nc()[0]
    nc = _CACHE["nc"]
    shared = {}
    for name, shp in PARAM_SHAPES:
        shared[name] = np.ascontiguousarray(np.asarray(inputs[name], dtype=np.float32).reshape(shp))
    in_maps = []
    for c in range(8):
        m = dict(shared)
        m["x"] = x[2 * c:2 * c + 2]
        in_maps.append(m)
    res = run_bass_kernel_spmd(nc, in_maps, core_ids=list(range(8)))
    out = np.empty((16, SEQ, D), np.float32)
    for c in range(8):
        out[2 * c:2 * c + 2] = np.asarray(res.results[c]["y"]).reshape(2, SEQ, D)
    return out
```

```python
import math
from contextlib import ExitStack
import numpy as np
import concourse.bass as bass
import concourse.mybir as mybir
from concourse.bass_utils import run_bass_kernel_spmd

F32 = mybir.dt.float32
BF16 = mybir.dt.bfloat16
I32 = mybir.dt.int32
AF = mybir.ActivationFunctionType
ALU = mybir.AluOpType
AX = mybir.AxisListType

D = 2048
NCOL = 8480
SEQ = 2048
NB = 2
TM = 128
NTM = SEQ // TM
NE = 32
DE = 512
TF = 512
NTF = NB * SEQ // TF
RMS_EPS = 1e-6
GN_EPS = 64e-5
DEC_K = math.exp(-0.5)
TWO_PI = 2.0 * math.pi


class Buf:
    __slots__ = ("name", "w", "r", "semcnt", "sem")

    def __init__(self, name):
        self.name = name
        self.w = None
        self.r = {}
        self.semcnt = 0
        self.sem = None


class Sched:
    ENG = ("pe", "act", "dve", "pool", "sp")
    SAME_SYNC = {"pe": False, "act": True, "dve": True, "pool": True, "sp": False}

    def __init__(self, nc, st, n_dma_sems):
        self.nc = nc
        self.q = {e: [] for e in self.ENG}
        self.cnt = {e: 0 for e in self.ENG}
        self.known = {e: {} for e in self.ENG}
        self.esem = {e: st.enter_context(nc.semaphore("s_" + e)) for e in self.ENG}
        self.dpool = [st.enter_context(nc.semaphore("d%d" % i)) for i in range(n_dma_sems)]
        self.dnext = 0
        self.chans = []
        self.ninst = 0
        self.pe_isa = 0
        self.pe_sem_val = 0
        self.pe_zone_done = 0

    def _deps(self, eng, r, w):
        need = {}
        kn = self.known[eng]
        me = ("eng", eng)

        def add(ev, kind):
            k, v = ev
            if k == me and not self.SAME_SYNC[eng]:
                return
            if kn.get(k, 0) >= v:
                return
            if need.get(k, 0) < v:
                need[k] = v

        for b in r:
            if b.w is not None:
                add(b.w, "raw")
        for b in w:
            if b.w is not None:
                add(b.w, "waw")
            for k, v in b.r.items():
                add((k, v), "war")
        for k, v in need.items():
            kn[k] = v
        return list(need.items())

    def op(self, eng, fn, r=(), w=(), chan=None, kind=None):
        waits = self._deps(eng, r, w)
        if eng == "pe":
            lk = getattr(self, "_pe_kind", None)
            import os as _osq
            _ser = getattr(self, "pe_serial", True)
            if lk is not None and (kind != lk or _ser) and self.cnt["pe"] > 0:
                k = ("eng", "pe")
                if self.known["pe"].get(k, 0) < self.cnt["pe"]:
                    self.known["pe"][k] = self.cnt["pe"]
                    waits = [wv for wv in waits if wv[0] != k] + [(k, self.cnt["pe"])]
            self._pe_kind = kind
        if chan is None:
            self.cnt[eng] += 1
            ev = (("eng", eng), self.cnt[eng])
        else:
            if chan.sem is None:
                chan.sem = self.dpool[self.dnext]
                self.dnext += 1
                self.chans.append(chan)
            chan.semcnt += 16
            ev = (("dma", chan), chan.semcnt)
        self.q[eng].append((waits, fn, ev))
        k, v = ev
        for b in r:
            if b.r.get(k, 0) < v:
                b.r[k] = v
        for b in w:
            b.w = ev
            b.r = {}
        return ev

    def dma(self, eng, out, in_, r, w, chan=None, **kw):
        ch = chan if chan is not None else w[0]
        return self.op(eng, lambda e: e.dma_start(out=out, in_=in_, **kw), r, w, chan=ch)

    def wait_events(self, eng, events):
        need = {}
        kn = self.known[eng]
        for k, v in events:
            if kn.get(k, 0) >= v:
                continue
            if need.get(k, 0) < v:
                need[k] = v
        for k, v in need.items():
            kn[k] = v
        if need:
            self.q[eng].append((list(need.items()), None, None))

    def all_events(self):
        evs = [(("eng", e), self.cnt[e]) for e in self.ENG if self.cnt[e] > 0]
        evs += [(("dma", c), c.semcnt) for c in self.chans]
        return evs

    def barrier(self):
        evs = self.all_events()
        for e in self.ENG:
            self.wait_events(e, evs)

    def _sem(self, k):
        return self.esem[k[1]] if k[0] == "eng" else k[1].sem

    def flush(self):
        nc = self.nc
        pew = set()
        for e_ in self.ENG:
            for waits, fn, ev in self.q[e_]:
                for k, v in waits:
                    if k == ("eng", "pe"):
                        pew.add(v)
        pe_ops = [ev[1] for waits, fn, ev in self.q["pe"] if fn is not None]
        if pe_ops:
            pew.add(pe_ops[-1])
        self._pew = pew
        with nc.Block() as block:
            def mk(e):
                def body(engh):
                    import os as _osp
                    for waits, fn, ev in self.q[e]:
                        for k, v in waits:
                            engh.wait_ge(self._sem(k), v)
                            self.ninst += 1
                            if e == "pe":
                                self.pe_isa += 1
                        if fn is None:
                            continue
                        if e == "pe":
                            _off = int(_osp.environ.get("PEOFF", "330"))
                            _half = int(_osp.environ.get("PEZONE", "0"))
                            _est = self.pe_isa + _off
                            _nb = (_est + _half) // 16384
                            if _nb > self.pe_zone_done:
                                self.pe_zone_done = _nb
                                for _ in range(2 * _half):
                                    engh.wait_ge(self.esem["pe"], 0)
                                self.pe_isa += 2 * _half
                            if (self.pe_isa + int(_osp.environ.get("PEPAD", "0"))) % 2 == 1:
                                engh.wait_ge(self.esem["pe"], 0)
                                self.pe_isa += 1
                            self.pe_isa += 2
                        ins = fn(engh)
                        ins.then_inc(self._sem(ev[0]), 16 if ev[0][0] == "dma" else 1)
                        self.ninst += 1
                return body

            block.tensor(mk("pe"))
            block.scalar(mk("act"))
            block.vector(mk("dve"))
            block.gpsimd(mk("pool"))
            block.sync(mk("sp"))
        if not hasattr(self, "stats"):
            self.stats = []
        self.stats.append({e: (len(self.q[e]), sum(len(w) for w, _, _ in self.q[e])) for e in self.ENG})
        self.q = {e: [] for e in self.ENG}


PARAM_SHAPES = [
    ("norm_mix_g", [D]), ("w_in", [D, NCOL]), ("rwkv_mu", [3360]), ("rwkv_w0", [1024]),
    ("rwkv_w_up", [64, 1024]), ("rwkv_a0", [1024]), ("rwkv_a_up", [64, 1024]), ("rwkv_g_up", [160, 1024]),
    ("rwkv_k_k", [1024]), ("rwkv_k_a", [1024]), ("rwkv_r_k", [1024]), ("rwkv_ln_w", [1024]),
    ("rwkv_ln_b", [1024]), ("rwkv_w_out", [1024, D]), ("s5_lam_re", [64, 64]), ("s5_lam_im", [64, 64]),
    ("s5_log_step", [64]), ("s5_b_re", [64, 64, 16]), ("s5_b_im", [64, 64, 16]), ("s5_c_re", [64, 16, 64]),
    ("s5_c_im", [64, 16, 64]), ("s5_d", [1024]), ("s5_w_glu_v", [1024, D]), ("s5_w_glu_g", [1024, D]),
    ("w_out", [D, D]), ("norm_ffn_g", [D]), ("router_group_w", [D, 4]), ("router_group_b", [4]),
    ("router_expert_w", [D, 32]), ("router_expert_b", [32]), ("moe_w_gate", [NE, D, DE]),
    ("moe_w_up", [NE, D, DE]), ("moe_w_down", [NE, DE, D]), ("norm_final_g", [D]),
]


class _Stop(Exception):
    pass


class _SkipM(Exception):
    pass


_R = {}


def build_nc(**kw):
    try:
        return _build_nc(**kw)
    except _Stop:
        return _R["nc"], _R["dbg"], _R["S"]


def _build_nc(ntm=NTM, ntf=NTF, dbg=(), stop=None, split=None):
    nc = bass.Bass("TRN2", target_bir_lowering=False)
    P = {}
    F_ONLY = ("moe_w_gate", "moe_w_up", "moe_w_down", "router_group_w", "router_group_b", "router_expert_w",
              "router_expert_b", "norm_ffn_g", "norm_final_g")
    if split == "M":
        xin = nc.dram_tensor("x", [NB, ntm * TM, D], F32, kind="ExternalInput").ap()
    elif split is None:
        xin = nc.dram_tensor("x", [NB, SEQ, D], F32, kind="ExternalInput").ap()
    for name, shp in PARAM_SHAPES:
        if split == "M" and name in F_ONLY:
            continue
        if split == "F" and name not in F_ONLY:
            continue
        P[name] = nc.dram_tensor(name, shp, F32, kind="ExternalInput").ap()
    if split == "M":
        yout = None
        x2_ext = nc.dram_tensor("x2", [NB, ntm * TM, D], F32, kind="ExternalOutput").ap()
        st_in = nc.dram_tensor("st_in", [128, 1024], F32, kind="ExternalInput").ap()
        s5c_in = nc.dram_tensor("s5c_in", [128, 128], F32, kind="ExternalInput").ap()
        carry_in = nc.dram_tensor("carry_in", [128, 54], F32, kind="ExternalInput").ap()
        st_out = nc.dram_tensor("st_out", [128, 1024], F32, kind="ExternalOutput").ap()
        s5c_out = nc.dram_tensor("s5c_out", [128, 128], F32, kind="ExternalOutput").ap()
        carry_out = nc.dram_tensor("carry_out", [128, 54], F32, kind="ExternalOutput").ap()
    elif split == "F":
        yout = nc.dram_tensor("y", [ntf * TF, D], F32, kind="ExternalOutput").ap()
    else:
        yout = nc.dram_tensor("y", [NB * SEQ, D], F32, kind="ExternalOutput").ap()
    dbg_out = {}

    def dram(name, shape, dt):
        return nc.dram_tensor(name, shape, dt, kind="Internal").ap()

    win_bf = dram("win_bf", [D, NCOL], BF16)
    wup_bf = dram("wup_bf", [64, 1024], BF16)
    aup_bf = dram("aup_bf", [64, 1024], BF16)
    gup_bf = dram("gup_bf", [160, 1024], BF16)
    wro_bf = dram("wro_bf", [1024, D], BF16)
    wgv_bf = dram("wgv_bf", [1024, D], BF16)
    wgg_bf = dram("wgg_bf", [1024, D], BF16)
    wo_bf = dram("wo_bf", [D, D], BF16)
    wrg_bf = dram("wrg_bf", [D, 4], BF16)
    wre_bf = dram("wre_bf", [D, 32], BF16)
    wg_bf = dram("wg_bf", [NE, D, DE], BF16)
    wu_bf = dram("wu_bf", [NE, D, DE], BF16)
    wd_bf = dram("wd_bf", [NE, DE, D], BF16)
    if split == "F":
        x2_dr = nc.dram_tensor("x2in", [ntf * TF, D], F32, kind="ExternalInput").ap()
    else:
        x2_dr = dram("x2_dr", [NB * SEQ, D], F32)

    top = ExitStack()
    S = Sched(nc, top, 96)

    _R["nc"] = nc
    _R["dbg"] = dbg_out
    _R["S"] = S

    _ck = {"n": 0}

    def ckpt(name):
        if stop == name:
            import os as _os9
            _ck["n"] += 1
            if _ck["n"] <= int(_os9.environ.get("STOPIT", "0")):
                return
            S.barrier()
            S.flush()
            raise _Stop()

    def tt(eng, out, in0, in1, op, r, w):
        return S.op(eng, lambda e: e.tensor_tensor(out=out, in0=in0, in1=in1, op=op), r, w)

    def ts(eng, out, in0, s1, s2, op0, op1, r, w):
        if s2 is None:
            return S.op(eng, lambda e: e.tensor_scalar(out=out, in0=in0, scalar1=s1, scalar2=None, op0=op0), r, w)
        return S.op(eng, lambda e: e.tensor_scalar(out=out, in0=in0, scalar1=s1, scalar2=s2, op0=op0, op1=op1), r, w)

    def stt(eng, out, in0, scalar, in1, op0, op1, r, w):
        return S.op(eng, lambda e: e.scalar_tensor_tensor(out=out, in0=in0, scalar=scalar, in1=in1, op0=op0, op1=op1), r, w)

    def actf(out, in_, func, r, w, bias=0.0, scale=1.0, accum=None):
        if accum is None:
            return S.op("act", lambda e: e.activation(out=out, in_=in_, func=func, bias=bias, scale=scale), r, w)
        return S.op("act", lambda e: e.activation(out=out, in_=in_, func=func, bias=bias, scale=scale, accum_out=accum), r, w)

    def cp(eng, out, in_, r, w):
        if eng == "act":
            return actf(out, in_, AF.Identity, r, w)
        return S.op(eng, lambda e: e.tensor_copy(out=out, in_=in_), r, w)

    def mm(out, lhsT, rhs, start, stop, r, w):
        kd = ("M", int(lhsT.shape[0]), int(lhsT.shape[-1]) if len(lhsT.shape) == 2 else -1)
        return S.op("pe", lambda e: e.matmul(out, lhsT, rhs, start=start, stop=stop), r, w, kind=kd)

    def tr(out, in_, ident, r, w):
        kd = ("T", int(in_.shape[0]), int(in_.shape[-1]))
        return S.op("pe", lambda e: e.transpose(out, in_, ident), r, w, kind=kd)

    def memset(eng, ap, val, w):
        return S.op(eng, lambda e: e.memset(ap, val), (), w)

    def scan(out, d0, d1, init, r, w):
        return S.op("dve", lambda e: e.tensor_tensor_scan(out=out, data0=d0, data1=d1, initial=init, op0=ALU.mult, op1=ALU.add), r, w)

    def recip(out, in_, r, w):
        return S.op("dve", lambda e: e.reciprocal(out=out, in_=in_), r, w)

    def rsum(out, in_, r, w):
        return S.op("dve", lambda e: e.reduce_sum(out=out, in_=in_, axis=AX.X), r, w)

    def rmax(out, in_, r, w):
        return S.op("dve", lambda e: e.reduce_max(out=out, in_=in_, axis=AX.X), r, w)

    def affsel(out, in_, pattern, cmp_op, fill, base, cm, r, w):
        return S.op("pool", lambda e: e.affine_select(out=out, in_=in_, pattern=pattern, compare_op=cmp_op, fill=fill, base=base, channel_multiplier=cm), r, w)

    def dbg_dump(name, sb_ap, shape, buf, dt=F32):
        if name not in dbg:
            return
        o = nc.dram_tensor("dbg_" + name, shape, dt, kind="ExternalOutput").ap()
        dbg_out[name] = o
        S.dma("sp", o, sb_ap, r=[buf], w=[Buf("dbgo_" + name)])

    def flat128(ap):
        n = 1
        for s in ap.shape:
            n *= s
        names = " ".join("a%d" % i for i in range(len(ap.shape)))
        f = ap.rearrange("%s -> (%s)" % (names, names)) if len(ap.shape) > 1 else ap
        return f.rearrange("(p f) -> p f", p=128)

    b_mixw = Buf("mixw")
    ch_mix = Buf("ch_mix")
    mix_list = [(win_bf, "w_in"), (wup_bf, "rwkv_w_up"), (aup_bf, "rwkv_a_up"), (gup_bf, "rwkv_g_up"),
                (wro_bf, "rwkv_w_out"), (wgv_bf, "s5_w_glu_v"), (wgg_bf, "s5_w_glu_g"), (wo_bf, "w_out"),
                (wrg_bf, "router_group_w"), (wre_bf, "router_expert_w")]
    mix_list = [(d_, P[n_]) for d_, n_ in mix_list if n_ in P]
    ev = None
    for dst, src in mix_list:
        if dst is win_bf:
            d2 = flat128(dst)
            s2 = flat128(src)
            nch = 8
            fw = d2.shape[1] // nch
            for c in range(nch):
                ev = S.dma("pool", d2[:, c * fw:(c + 1) * fw], s2[:, c * fw:(c + 1) * fw], r=[], w=[], chan=ch_mix)
        else:
            ev = S.dma("pool", flat128(dst), flat128(src), r=[], w=[], chan=ch_mix)
    b_mixw.w = ev
    b_moew = []
    for g4 in range(4 if (ntf > 0 and split != "M") else 0):
        ch = Buf("ch_moe%d" % g4)
        bb = Buf("moew%d" % g4)
        for e in range(g4 * 8, g4 * 8 + 8):
            for dst, src in ((wg_bf, P["moe_w_gate"]), (wu_bf, P["moe_w_up"]), (wd_bf, P["moe_w_down"])):
                ev = S.dma("pool", flat128(dst[e]), flat128(src[e]), r=[], w=[], chan=ch)
        bb.w = ev
        b_moew.append(bb)

    S.barrier()
    ckpt("precast")
    def sb(stk, name, shape, dt=F32):
        return stk.enter_context(nc.sbuf_tensor(name, shape, dt))

    ident = sb(top, "ident", [128, 128], BF16)
    b_ident = Buf("ident")
    identf = sb(top, "identf", [128, 128], F32)
    b_identf = Buf("identf")
    memset("pool", identf[:], 0.0, [b_identf])
    affsel(identf[:], identf[:], [[-1, 128]], ALU.not_equal, 1.0, 0, 1, [b_identf], [b_identf])
    cp("dve", ident[:], identf[:], [b_identf], [b_ident])

    pbank = [top.enter_context(nc.psum_tensor("pb%d" % i, [128, 512], F32)) for i in range(8)]
    b_pb = [Buf("pb%d" % i) for i in range(8)]
    pstate = {"i": 0}

    def nbank():
        i = pstate["i"]
        pstate["i"] = (i + 1) % 8
        return pbank[i], b_pb[i]

    def colvec(stk, name, src_ap, n):
        t = sb(stk, name, [128, n], F32)
        b = Buf(name)
        S.dma("sp", t[:], src_ap.rearrange("(c p) -> p c", p=128), r=[], w=[b], allow_slow_non_contiguous=True)
        return t, b

    def bcast_rows(stk, name, src_ap, n, parts=128):
        t = sb(stk, name, [parts, n], F32)
        b = Buf(name)
        S.dma("sp", t[:], src_ap.partition_broadcast(parts), r=[], w=[b])
        return t, b

    try:
      with ExitStack() as pm:
          if split == "F":
              raise _SkipM()
          g1col, b_g1 = colvec(pm, "g1col", P["norm_mix_g"], 16)
          mucol = sb(pm, "mucol", [128, 27], F32)
          b_mu = Buf("mucol")
          memset("pool", mucol[:], 0.0, [b_mu])
          S.dma("sp", mucol[:, 0:26], P["rwkv_mu"][0:3328].rearrange("(c p) -> p c", p=128), r=[], w=[b_mu], allow_slow_non_contiguous=True)
          S.dma("sp", mucol[0:32, 26:27], P["rwkv_mu"][3328:3360].rearrange("(c p) -> p c", p=32), r=[], w=[b_mu], allow_slow_non_contiguous=True)
          ommcol = sb(pm, "ommcol", [128, 27], F32)
          b_omm = Buf("ommcol")
          ts("dve", ommcol[:], mucol[:], -1.0, 1.0, ALU.mult, ALU.add, [b_mu], [b_omm])
          w0col, b_w0 = colvec(pm, "w0col", P["rwkv_w0"], 8)
          a0col, b_a0 = colvec(pm, "a0col", P["rwkv_a0"], 8)
          kkcol, b_kk = colvec(pm, "kkcol", P["rwkv_k_k"], 8)
          kacol, b_ka = colvec(pm, "kacol", P["rwkv_k_a"], 8)
          rkcol, b_rk = colvec(pm, "rkcol", P["rwkv_r_k"], 8)
          dcol, b_dc = colvec(pm, "dcol", P["s5_d"], 8)
          omka = sb(pm, "omka", [128, 8], F32)
          b_omka = Buf("omka")
          ts("dve", omka[:], kacol[:], -1.0, 1.0, ALU.mult, ALU.add, [b_ka], [b_omka])
          lnw, b_lnw = bcast_rows(pm, "lnw", P["rwkv_ln_w"], 1024)
          lnb, b_lnb = bcast_rows(pm, "lnb", P["rwkv_ln_b"], 1024)

          mnb = sb(pm, "mnb", [64, 128], F32)
          b_mnb = Buf("mnb")
          memset("pool", mnb[:], 1.0, [b_mnb])
          affsel(mnb[:, 0:64], mnb[:, 0:64], [[1, 64]], ALU.is_gt, 0.0, 0, -1, [b_mnb], [b_mnb])
          affsel(mnb[:, 64:128], mnb[:, 64:128], [[1, 64]], ALU.is_ge, 0.0, 0, -1, [b_mnb], [b_mnb])
          mq = sb(pm, "mq", [64, 64], F32)
          b_mq = Buf("mq")
          memset("pool", mq[:], 1.0, [b_mq])
          affsel(mq[:], mq[:], [[-1, 64]], ALU.is_gt, 0.0, 0, 1, [b_mq], [b_mq])
          identb64 = ident[0:64, 0:64]
          onesblk = sb(pm, "onesblk", [128, 128], BF16)
          b_ob = Buf("onesblk")
          memset("pool", onesblk[:], 0.0, [b_ob])
          memset("pool", onesblk[0:64, 0:64], 1.0, [b_ob])
          memset("pool", onesblk[64:128, 64:128], 1.0, [b_ob])
          ind2 = sb(pm, "ind2", [128, 2], BF16)
          b_ind2 = Buf("ind2")
          memset("pool", ind2[:], 0.0, [b_ind2])
          memset("pool", ind2[0:64, 0:1], 1.0, [b_ind2])
          memset("pool", ind2[64:128, 1:2], 1.0, [b_ind2])
          scmask = sb(pm, "scmask", [128, 256], F32)
          b_scm = Buf("scmask")
          memset("pool", scmask[:], 1.0, [b_scm])
          memset("pool", scmask[:].rearrange("p (k t) -> p k t", t=64)[:, :, 0:1], 0.0, [b_scm])

          wa_up = sb(pm, "wa_up", [128, 1024], BF16)
          b_waup = Buf("wa_up")
          b_waup2 = Buf("wa_up2")
          S.dma("sp", wa_up[0:64, :], wup_bf[:, :], r=[b_mixw], w=[b_waup])
          S.dma("sp", wa_up[64:128, :], aup_bf[:, :], r=[b_mixw], w=[b_waup2])
          gup1 = sb(pm, "gup1", [128, 1024], BF16)
          b_gup1 = Buf("gup1")
          S.dma("sp", gup1[:], gup_bf[0:128, :], r=[b_mixw], w=[b_gup1])
          gup2 = sb(pm, "gup2", [32, 1024], BF16)
          b_gup2 = Buf("gup2")
          S.dma("sp", gup2[:], gup_bf[128:160, :], r=[b_mixw], w=[b_gup2])

          ckpt("consts")
          rho = sb(pm, "rho", [128, 32], F32); b_rho = Buf("rho")
          theta = sb(pm, "theta", [128, 32], F32); b_theta = Buf("theta")
          Bl = sb(pm, "Bl", [128, 32, 2, 128], BF16); b_Bl = Buf("Bl")
          memset("pool", Bl[:], 0.0, [b_Bl])
          Cl = sb(pm, "Cl", [128, 32, 2, 128], BF16); b_Cl = Buf("Cl")
          tabS = sb(pm, "tabS", [128, 32, 128], BF16); b_tabS = Buf("tabS")
          tabC = sb(pm, "tabC", [128, 32, 128], BF16); b_tabC = Buf("tabC")
          with ExitStack() as s5s:
              ones256 = sb(s5s, "ones256", [128, 256], F32)
              b_ones = Buf("ones256")
              memset("pool", ones256[:], 1.0, [b_ones])
              lre = sb(s5s, "lre", [128, 32], F32); b_lre = Buf("lre")
              lim = sb(s5s, "lim", [128, 32], F32); b_lim = Buf("lim")
              lst = sb(s5s, "lst", [128, 32], F32); b_lst = Buf("lst")
              S.dma("sp", lre[:], P["s5_lam_re"].rearrange("(t g) p -> (g p) t", g=2), r=[], w=[b_lre], allow_slow_non_contiguous=True)
              S.dma("sp", lim[:], P["s5_lam_im"].rearrange("(t g) p -> (g p) t", g=2), r=[], w=[b_lim], allow_slow_non_contiguous=True)
              ls2 = P["s5_log_step"].rearrange("(t g) -> g t", g=2)
              b_lst2 = Buf("lst2")
              S.dma("sp", lst[0:64, :], ls2[0].partition_broadcast(64), r=[], w=[b_lst], allow_slow_non_contiguous=True)
              S.dma("sp", lst[64:128, :], ls2[1].partition_broadcast(64), r=[], w=[b_lst2], allow_slow_non_contiguous=True)
              step = sb(s5s, "step", [128, 32], F32); b_step = Buf("step")
              actf(step[:], lst[:], AF.Exp, [b_lst, b_lst2], [b_step])
              tmp32 = sb(s5s, "tmp32", [128, 32], F32); b_tmp32 = Buf("tmp32")
              tt("dve", tmp32[:], lre[:], step[:], ALU.mult, [b_lre, b_step], [b_tmp32])
              actf(rho[:], tmp32[:], AF.Exp, [b_tmp32], [b_rho])
              tt("dve", theta[:], lim[:], step[:], ALU.mult, [b_lim, b_step], [b_theta])

              def sincos(out_s, b_out_s, out_c, b_out_c, arg, b_arg, shape, nm):
                  u = sb(s5s, "sc_u" + nm, shape, F32); b_u = Buf("sc_u" + nm)
                  ki = sb(s5s, "sc_k" + nm, shape, I32); b_k = Buf("sc_k" + nm)
                  kf = sb(s5s, "sc_f" + nm, shape, F32); b_kf = Buf("sc_f" + nm)
                  for which, out, b_out in ((0, out_s, b_out_s), (1, out_c, b_out_c)):
                      ts("dve", u[:], arg, 1.0 / TWO_PI, 0.25 * which, ALU.mult, ALU.add, [b_arg], [b_u])
                      cp("dve", ki[:], u[:], [b_u], [b_k])
                      cp("dve", kf[:], ki[:], [b_k], [b_kf])
                      tt("dve", u[:], u[:], kf[:], ALU.subtract, [b_u, b_kf], [b_u])
                      ts("dve", kf[:], u[:], 0.5, None, ALU.is_gt, None, [b_u], [b_kf])
                      tt("dve", u[:], u[:], kf[:], ALU.subtract, [b_u, b_kf], [b_u])
                      ts("dve", kf[:], u[:], -0.5, None, ALU.is_lt, None, [b_u], [b_kf])
                      tt("dve", u[:], u[:], kf[:], ALU.add, [b_u, b_kf], [b_u])
                      actf(out, u[:], AF.Sin, [b_u], [b_out], scale=TWO_PI)

              sth = sb(s5s, "sth", [128, 32], F32); b_sth = Buf("sth")
              cth = sb(s5s, "cth", [128, 32], F32); b_cth = Buf("cth")
              sincos(sth[:], b_sth, cth[:], b_cth, theta[:], b_theta, [128, 32], "a")
              nre = sb(s5s, "nre", [128, 32], F32); b_nre = Buf("nre")
              nim = sb(s5s, "nim", [128, 32], F32); b_nim = Buf("nim")
              tt("dve", nre[:], rho[:], cth[:], ALU.mult, [b_rho, b_cth], [b_nre])
              ts("dve", nre[:], nre[:], -1.0, None, ALU.add, None, [b_nre], [b_nre])
              tt("dve", nim[:], rho[:], sth[:], ALU.mult, [b_rho, b_sth], [b_nim])
              den = sb(s5s, "den", [128, 32], F32); b_den = Buf("den")
              t2 = sb(s5s, "t2", [128, 32], F32); b_t2 = Buf("t2")
              tt("dve", den[:], lre[:], lre[:], ALU.mult, [b_lre], [b_den])
              tt("dve", t2[:], lim[:], lim[:], ALU.mult, [b_lim], [b_t2])
              tt("dve", den[:], den[:], t2[:], ALU.add, [b_den, b_t2], [b_den])
              recip(den[:], den[:], [b_den], [b_den])
              cre = sb(s5s, "cre", [128, 32], F32); b_cre = Buf("cre")
              cim = sb(s5s, "cim", [128, 32], F32); b_cim = Buf("cim")
              tt("dve", cre[:], nre[:], lre[:], ALU.mult, [b_nre, b_lre], [b_cre])
              tt("dve", t2[:], nim[:], lim[:], ALU.mult, [b_nim, b_lim], [b_t2])
              tt("dve", cre[:], cre[:], t2[:], ALU.add, [b_cre, b_t2], [b_cre])
              tt("dve", cre[:], cre[:], den[:], ALU.mult, [b_cre, b_den], [b_cre])
              tt("dve", cim[:], nim[:], lre[:], ALU.mult, [b_nim, b_lre], [b_cim])
              tt("dve", t2[:], nre[:], lim[:], ALU.mult, [b_nre, b_lim], [b_t2])
              tt("dve", cim[:], cim[:], t2[:], ALU.subtract, [b_cim, b_t2], [b_cim])
              tt("dve", cim[:], cim[:], den[:], ALU.mult, [b_cim, b_den], [b_cim])
              bnr = sb(s5s, "bnr", [128, 32, 16], F32); b_bnr = Buf("bnr")
              bni = sb(s5s, "bni", [128, 32, 16], F32); b_bni = Buf("bni")
              S.dma("sp", bnr[:], P["s5_b_re"].rearrange("(t g) p h -> (g p) t h", g=2), r=[], w=[b_bnr], allow_slow_non_contiguous=True)
              S.dma("sp", bni[:], P["s5_b_im"].rearrange("(t g) p h -> (g p) t h", g=2), r=[], w=[b_bni], allow_slow_non_contiguous=True)
              bbr = sb(s5s, "bbr", [128, 32, 16], F32); b_bbr = Buf("bbr")
              bbi = sb(s5s, "bbi", [128, 32, 16], F32); b_bbi = Buf("bbi")
              t3 = sb(s5s, "t3", [128, 32, 16], F32); b_t3 = Buf("t3")
              crb = cre[:].unsqueeze(2).broadcast_to([128, 32, 16])
              cib = cim[:].unsqueeze(2).broadcast_to([128, 32, 16])
              tt("dve", bbr[:], bnr[:], crb, ALU.mult, [b_bnr, b_cre], [b_bbr])
              tt("dve", t3[:], bni[:], cib, ALU.mult, [b_bni, b_cim], [b_t3])
              tt("dve", bbr[:], bbr[:], t3[:], ALU.subtract, [b_bbr, b_t3], [b_bbr])
              tt("dve", bbi[:], bni[:], crb, ALU.mult, [b_bni, b_cre], [b_bbi])
              tt("dve", t3[:], bnr[:], cib, ALU.mult, [b_bnr, b_cim], [b_t3])
              tt("dve", bbi[:], bbi[:], t3[:], ALU.add, [b_bbi, b_t3], [b_bbi])
              bex = sb(s5s, "bex", [128, 2, 32, 64], BF16); b_bex = Buf("bex")
              memset("pool", bex[:], 0.0, [b_bex])
              for ri, (src, bsrc) in enumerate(((bbr, b_bbr), (bbi, b_bbi))):
                  for tq in range(4):
                      for g2 in range(2):
                          c0 = ((2 * tq + g2) % 4) * 16
                          dst = bex[g2 * 64:(g2 + 1) * 64, ri].rearrange("p (a b) c -> p a b c", b=4)[:, :, tq, c0:c0 + 16]
                          s_ = src[g2 * 64:(g2 + 1) * 64].rearrange("p (a b) c -> p a b c", b=4)[:, :, tq, :]
                          cp("dve", dst, s_, [bsrc], [b_bex])
              for t in range(32):
                  po = 64 * ((t % 4) // 2)
                  pbt, bpb = nbank()
                  pv = pbt[:].bitcast(BF16)
                  for ri in range(2):
                      tr(pv[0:64, ri * 128:(ri + 1) * 128], bex[:, ri, t, :], ident[:], [b_bex, b_ident], [bpb])
                  cp("dve", Bl[po:po + 64, t, :, :], pv[0:64, 0:256].rearrange("p (r q) -> p r q", r=2), [bpb], [b_Bl])
              cnr = sb(s5s, "cnr", [128, 8, 64], F32); b_cnr = Buf("cnr")
              cni = sb(s5s, "cni", [128, 8, 64], F32); b_cni = Buf("cni")
              S.dma("sp", cnr[:], P["s5_c_re"].rearrange("(m g) h p -> (g h) m p", g=8), r=[], w=[b_cnr])
              S.dma("sp", cni[:], P["s5_c_im"].rearrange("(m g) h p -> (g h) m p", g=8), r=[], w=[b_cni])
              gm = sb(s5s, "gm", [128, 8], F32); b_gm = Buf("gm")
              memset("pool", gm[:], 1.0, [b_gm])
              affsel(gm[:], gm[:], [[-16, 8]], ALU.is_ge, 0.0, 0, 1, [b_gm], [b_gm])
              affsel(gm[:], gm[:], [[16, 8]], ALU.is_ge, 0.0, 15, -1, [b_gm], [b_gm])
              cex_ = sb(s5s, "cexp", [128, 2, 128], BF16); b_cex = Buf("cexp")
              for t in range(32):
                  m_ = t // 4
                  for ri, (src, bsrc, sgn) in enumerate(((cnr, b_cnr, 1.0), (cni, b_cni, -1.0))):
                      for g2 in range(2):
                          q8 = (2 * t + g2) % 8
                          ts("dve", cex_[:, ri, g2 * 64:(g2 + 1) * 64], src[:, m_, :], gm[:, q8:q8 + 1], sgn, ALU.mult, ALU.mult, [bsrc, b_gm], [b_cex])
                  pbt, bpb = nbank()
                  pv = pbt[:].bitcast(BF16)
                  for ri in range(2):
                      tr(pv[:, ri * 128:(ri + 1) * 128], cex_[:, ri, :], ident[:], [b_cex, b_ident], [bpb])
                  cp("dve", Cl[:, t, :, :], pv[:, 0:256].rearrange("p (r q) -> p r q", r=2), [bpb], [b_Cl])
              tau = sb(s5s, "tau", [128, 128], F32); b_tau = Buf("tau")
              scan(tau[:], ones256[:, 0:128], ones256[:, 0:128], 0.0, [b_ones], [b_tau])
              targ = sb(s5s, "targ", [128, 32, 128], F32); b_targ = Buf("targ")
              tt("dve", targ[:], tau[:].unsqueeze(1).broadcast_to([128, 32, 128]), theta[:].unsqueeze(2).broadcast_to([128, 32, 128]), ALU.mult, [b_tau, b_theta], [b_targ])
              sincos(tabS[:], b_tabS, tabC[:], b_tabC, targ[:], b_targ, [128, 32, 128], "b")
              S.barrier()
              S.flush()
          dbg_dump("rho", rho[:], [128, 32], b_rho)
          dbg_dump("theta", theta[:], [128, 32], b_theta)
          dbg_dump("tabS", tabS[:], [128, 32, 128], b_tabS, BF16)
          dbg_dump("tabC", tabC[:], [128, 32, 128], b_tabC, BF16)
          dbg_dump("Bl", Bl[:], [128, 32, 2, 128], b_Bl, BF16)
          dbg_dump("Cl", Cl[:], [128, 32, 2, 128], b_Cl, BF16)

          ckpt("s5setup")
          ST = sb(pm, "ST", [128, 8, NB, 64], F32); b_ST = [Buf("ST%d" % m) for m in range(8)]
          STb = sb(pm, "STb", [64, 16, NB, 64], BF16); b_STb = [Buf("STb%d" % m) for m in range(8)]
          if split == "M":
              S.dma("sp", ST[:].rearrange("p a b c -> p (a b c)"), st_in, r=[], w=b_ST, chan=Buf("ch_stin"))
              for m_ in range(8):
                  cp("act", STb[:, m_ * 2, :, :], ST[0:64, m_, :, :], [b_ST[m_]], [b_STb[m_]])
                  cp("dve", STb[:, m_ * 2 + 1, :, :], ST[64:128, m_, :, :], [b_ST[m_]], [b_STb[m_]])
          else:
              memset("pool", ST[:], 0.0, b_ST)
              memset("pool", STb[:], 0.0, b_STb)
          s5c = sb(pm, "s5c", [128, 32, 2, NB], F32); b_s5c = [Buf("s5c%d" % t) for t in range(32)]
          if split == "M":
              S.dma("sp", s5c[:].rearrange("p a b c -> p (a b c)"), s5c_in, r=[], w=b_s5c, chan=Buf("ch_s5in"))
          else:
              memset("pool", s5c[:], 0.0, b_s5c)
          carry = sb(pm, "carry", [128, 27, NB], F32); b_carry = [Buf("carry%d" % j) for j in range(27)]
          if split == "M":
              S.dma("sp", carry[:].rearrange("p a b -> p (a b)"), carry_in, r=[], w=b_carry, chan=Buf("ch_cin"))
          else:
              memset("pool", carry[:], 0.0, b_carry)

          xld_ = sb(pm, "xld", [128, D], F32); xld = [xld_, xld_]; b_xld_ = Buf("xld"); b_xld = [b_xld_, b_xld_]
          xsb = [sb(pm, "xsb%d" % i, [128, D], BF16) for i in range(2)]; b_xsb = [Buf("xsb%d" % i) for i in range(2)]
          stat = sb(pm, "stat", [128, 8], F32); b_stat = [Buf("stat%d" % i) for i in range(2)]
          hT = sb(pm, "hT", [128, 16, 256], BF16); b_hT = Buf("hT")
          NWS = 2
          wsl = [sb(pm, "wsl%d" % i, [128, 16, 256], BF16) for i in range(NWS)]; b_wsl = [Buf("wsl%d" % i) for i in range(NWS)]
          wst = {"i": 0}

          def wslot():
              i = wst["i"]
              wst["i"] = (i + 1) % NWS
              return wsl[i], b_wsl[i]

          NSC = 15
          scr = [sb(pm, "scr%d" % i, [128, 256], F32) for i in range(NSC)]; b_scr = [Buf("scr%d" % i) for i in range(NSC)]
          sst = {"i": 0}

          def nscr(i=None):
              if i is None:
                  i = sst["i"]
                  sst["i"] = (i + 1) % NSC
              return scr[i], b_scr[i]

          def scr_reset():
              sst["i"] = 0

          lA = sb(pm, "lA", [128, 256], BF16); b_lA = Buf("lA")
          lA2 = sb(pm, "lA2", [128, 256], BF16); b_lA2 = Buf("lA2")
          memset("pool", lA[:], 0.0, [b_lA])
          memset("pool", lA2[:], 0.0, [b_lA2])
          lB1 = sb(pm, "lB1", [128, 256], BF16); b_lB1 = Buf("lB1")
          lB2 = sb(pm, "lB2", [32, 256], BF16); b_lB2 = Buf("lB2")
          rkv = [sb(pm, "rkv%d" % i, [128, 3, 256], F32) for i in range(2)]; b_rkv = [Buf("rkv%d" % i) for i in range(2)]
          vTb = [sb(pm, "vTb%d" % i, [128, 256], BF16) for i in range(2)]; b_vTb = [Buf("vTb%d" % i) for i in range(2)]
          ARh = sb(pm, "ARh", [64, 2, 4, 2, 64], BF16); b_ARh = Buf("ARh")
          BKh = sb(pm, "BKh", [64, 2, 2, 256], BF16); b_BKh = Buf("BKh")
          tokBK = [sb(pm, "tokBK%d" % i, [64, 4, 2, 128], BF16) for i in range(2)]; b_tokBK = [Buf("tokBK%d" % i) for i in range(2)]
          tokV = [sb(pm, "tokV%d" % i, [64, 4, 128], BF16) for i in range(2)]; b_tokV = [Buf("tokV%d" % i) for i in range(2)]
          tokV2 = [sb(pm, "tokV2%d" % i, [128, NB, 128], F32) for i in range(2)]; b_tokV2 = [Buf("tokV2%d" % i) for i in range(2)]
          gtok = [sb(pm, "gtok%d" % i, [128, NB, 128], F32) for i in range(2)]; b_gtok = [Buf("gtok%d" % i) for i in range(2)]
          bon = [sb(pm, "bon%d" % i, [128, NB, 2], F32) for i in range(2)]; b_bon = [Buf("bon%d" % i) for i in range(2)]
          cL = [sb(pm, "cL%d" % i, [128, 4], F32) for i in range(2)]; b_cL = [Buf("cL%d" % i) for i in range(2)]
          NBR = sb(pm, "NBR", [64, 8, 128], BF16); b_NBR = Buf("NBR")
          KAR = sb(pm, "KAR", [64, 8, 128], BF16); b_KAR = Buf("KAR")
          Pm = [sb(pm, "Pm%d" % i, [64, 8, 64], BF16) for i in range(2)]; b_Pm = [Buf("Pm%d" % i) for i in range(2)]
          Qm = [sb(pm, "Qm%d" % i, [64, 8, 64], BF16) for i in range(2)]; b_Qm = [Buf("Qm%d" % i) for i in range(2)]
          Am = [sb(pm, "Am%d" % i, [64, 8, 64], F32) for i in range(2)]; b_Am = [Buf("Am%d" % i) for i in range(2)]
          Amb = sb(pm, "Amb", [64, 8, 64], BF16); b_Amb = Buf("Amb")
          Xs = sb(pm, "Xs", [64, 4, 64], BF16); b_Xs = Buf("Xs")
          Us = sb(pm, "Us", [64, NB, 128], BF16); b_Us = Buf("Us")
          Ytok = sb(pm, "Ytok", [128, NB, 128], F32); b_Ytok = Buf("Ytok")
          gnt = [sb(pm, "gnt%d" % i, [128, NB, 128], F32) for i in range(2)]; b_gnt = [Buf("gnt%d" % i) for i in range(2)]
          gst = sb(pm, "gst", [128, 8], F32); b_gst = Buf("gst")
          ygtok = sb(pm, "ygtok", [128, NB, 128], BF16); b_ygtok = Buf("ygtok")
          ygT = sb(pm, "ygT", [128, 8, 256], BF16); b_ygT = Buf("ygT")
          uT = sb(pm, "uT", [128, 8, 256], BF16); b_uT = Buf("uT")
          zT = sb(pm, "zT", [128, 8, 256], BF16); b_zT = Buf("zT")
          xs5_ = sb(pm, "xs5", [128, 4, 2, 256], BF16); xs5 = [xs5_, xs5_]; b_xs5_ = Buf("xs5"); b_xs5 = [b_xs5_, b_xs5_]
          mixT = sb(pm, "mixT", [128, 16, 256], BF16); b_mixT = Buf("mixT")
          wsm = [sb(pm, "wsm%d" % i, [128, 8, 128], BF16) for i in range(4)]; b_wsm = [Buf("wsm%d" % i) for i in range(4)]
          wsmst = {"i": 0}

          def wsmslot():
              i = wsmst["i"]
              wsmst["i"] = (i + 1) % 4
              return wsm[i], b_wsm[i]

          xres = [sb(pm, "xres%d" % i, [128, 256], F32) for i in range(2)]; b_xres = [Buf("xres%d" % i) for i in range(2)]
          x2o = [sb(pm, "x2o%d" % i, [128, 256], F32) for i in range(2)]; b_x2o = [Buf("x2o%d" % i) for i in range(2)]
          b_x2st = [Buf("x2st%d" % i) for i in range(2)]
          b_x2dr = Buf("x2dr")

          print("phase M sbuf remaining", nc.sbuf_bytes_remaining, flush=True)
          win_v = win_bf.rearrange("(kc p) c -> p kc c", p=128)

          def load_w(c0, ncols):
              w_, bw = wslot()
              S.dma("sp", w_[:, :, 0:ncols], win_v[:, :, c0:c0 + ncols], r=[b_mixw], w=[bw])
              return w_, bw

          def proj(w_, bw, wc0, ncols, out_ps, bps):
              for kc in range(16):
                  mm(out_ps, w_[:, kc, wc0:wc0 + ncols], hT[:, kc, :], kc == 0, kc == 15, [bw, b_hT], [bps])

          def shift_evac(ps, bps, j, rows, out, bout, eng2="dve"):
              tmp, btmp = nscr()
              p3 = ps.rearrange("p (b t) -> p b t", b=NB)
              t3_ = tmp[0:rows, :].rearrange("p (b t) -> p b t", b=NB)
              ts("dve", t3_[:, :, 1:TM], p3[:, :, 0:TM - 1], mucol[0:rows, j:j + 1], None, ALU.mult, None, [bps, b_mu], [btmp])
              ts("dve", t3_[:, :, 0:1], carry[0:rows, j, :].unsqueeze(2), mucol[0:rows, j:j + 1], None, ALU.mult, None, [b_carry[j], b_mu], [btmp])
              cp("act", carry[0:rows, j, :].unsqueeze(2), p3[:, :, TM - 1:TM], [bps], [b_carry[j]])
              stt(eng2, out, ps, ommcol[0:rows, j:j + 1], tmp[0:rows, :], ALU.mult, ALU.add, [bps, b_omm, btmp], [bout])

          for it in range(ntm):
              t0 = it * TM
              import os as _os7
              if _os7.environ.get("T0ZERO", "") == "1":
                  t0 = 0
              pstate["i"] = 0
              for b in range(NB):
                  S.dma("sp", xld[b][:], xin[b, t0:t0 + TM, :], r=[], w=[b_xld[b]])
                  actf(xsb[b][:], xld[b][:], AF.Square, [b_xld[b]], [b_xsb[b], b_stat[b]], accum=stat[:, 4 * b:4 * b + 1])
                  actf(stat[:, 4 * b + 1:4 * b + 2], stat[:, 4 * b:4 * b + 1], AF.Sqrt, [b_stat[b]], [b_stat[b]], bias=RMS_EPS, scale=1.0 / D)
                  recip(stat[:, 4 * b + 2:4 * b + 3], stat[:, 4 * b + 1:4 * b + 2], [b_stat[b]], [b_stat[b]])
                  actf(xsb[b][:], xld[b][:], AF.Identity, [b_xld[b], b_stat[b]], [b_xsb[b]], scale=stat[:, 4 * b + 2:4 * b + 3])
                  for half in range(2):
                      pbt, bpb = nbank()
                      pv = pbt[:].bitcast(BF16)
                      for k8 in range(8):
                          kc = half * 8 + k8
                          tr(pv[:, k8 * 128:(k8 + 1) * 128], xsb[b][:, kc * 128:(kc + 1) * 128], ident[:], [b_xsb[b], b_ident], [bpb])
                      tt("dve", hT[:, half * 8:half * 8 + 8, b * TM:(b + 1) * TM], pv[:, :].rearrange("p (k t) -> p k t", k=8),
                         g1col[:, half * 8:half * 8 + 8].unsqueeze(2).broadcast_to([128, 8, TM]), ALU.mult, [bpb, b_g1], [b_hT])
              if it == 0:
                  dbg_dump("hT", hT[:], [128, 16, 256], b_hT, BF16)

              ckpt("stage1")
              scr_reset()
              w_, bw = load_w(3072, 256)
              pbt, bpb = nbank()
              proj(w_, bw, 0, 128, pbt[:, 0:256], bpb)
              tA, btA = nscr()
              shift_evac(pbt[:, 0:256], bpb, 24, 128, tA[:], btA)
              actf(lA[0:64, :], tA[0:64, :], AF.Tanh, [btA], [b_lA])
              cp("dve", lA2[64:128, :], tA[64:128, :], [btA], [b_lA2])
              pbt, bpb = nbank()
              proj(w_, bw, 128, 128, pbt[:, 0:256], bpb)
              tB, btB = nscr()
              shift_evac(pbt[:, 0:256], bpb, 25, 128, tB[:], btB)
              actf(lB1[:], tB[:], AF.Sigmoid, [btB], [b_lB1])
              w_, bw = load_w(3328, 32)
              pbt, bpb = nbank()
              proj(w_, bw, 0, 32, pbt[0:32, 0:256], bpb)
              tB2, btB2 = nscr()
              shift_evac(pbt[0:32, 0:256], bpb, 26, 32, tB2[0:32, :], btB2)
              actf(lB2[:], tB2[0:32, :], AF.Sigmoid, [btB2], [b_lB2])

              ckpt("lora")
              for m in range(8):
                  pp = m % 2
                  scr_reset()
                  R3 = rkv[pp]; bR3 = b_rkv[pp]
                  wr_ = None
                  for which, c0 in enumerate((m * 128, 1024 + m * 128, 2048 + m * 128)):
                      w_, bw = load_w(c0, 128)
                      pbt, bpb = nbank()
                      proj(w_, bw, 0, 128, pbt[:, 0:256], bpb)
                      shift_evac(pbt[:, 0:256], bpb, which * 8 + m, 128, R3[:, which, :], bR3)
                  if m == 0: ckpt("rw_proj")
                  rT = R3[:, 0, :]; kT = R3[:, 1, :]; vT = R3[:, 2, :]
                  cp("act", vTb[pp][:], vT, [bR3], [b_vTb[pp]])
                  pbt, bpb = nbank()
                  mm(pbt[:, 0:256], wa_up[:, m * 128:(m + 1) * 128], lA[:, :], True, True, [b_waup, b_waup2, b_lA], [bpb])
                  if m == 0: ckpt("rw_m1")
                  mm(pbt[:, 256:512], wa_up[:, m * 128:(m + 1) * 128], lA2[:, :], True, True, [b_waup, b_waup2, b_lA2], [bpb])
                  if m == 0: ckpt("rw_m2")
                  sgw, bsgw = nscr()
                  aic, baic = nscr()
                  actf(sgw[:], pbt[:, 0:256], AF.Sigmoid, [bpb, b_w0], [bsgw], bias=w0col[:, m:m + 1])
                  actf(aic[:], pbt[:, 256:512], AF.Sigmoid, [bpb, b_a0], [baic], bias=a0col[:, m:m + 1])
                  if m == 0: ckpt("rw_l1")
                  pbt, bpb = nbank()
                  for b in range(NB):
                      mm(pbt[:, b * 128:(b + 1) * 128], lB1[:, b * TM:(b + 1) * TM], gup1[:, m * 128:(m + 1) * 128], True, False, [b_lB1, b_gup1], [bpb])
                      mm(pbt[:, b * 128:(b + 1) * 128], lB2[:, b * TM:(b + 1) * TM], gup2[:, m * 128:(m + 1) * 128], False, True, [b_lB2, b_gup2], [bpb])
                  cp("act", gtok[pp][:], pbt[:, 0:256].rearrange("p (b f) -> p b f", b=NB), [bpb], [b_gtok[pp]])
                  if m == 0: ckpt("rw_lora")
                  kk, bkk = nscr()
                  kk2, bkk2 = nscr()
                  ts("dve", kk[:], kT, kkcol[:, m:m + 1], None, ALU.mult, None, [bR3, b_kk], [bkk])
                  kk2b = kk2[:].bitcast(BF16)[:, 0:256]
                  tt("pool", kk2b, kk[:], kk[:], ALU.mult, [bkk], [bkk2])
                  pbt, bpb = nbank()
                  mm(pbt[:, 0:256], onesblk[:], kk2b, True, True, [b_ob, bkk2], [bpb])
                  rn, brn = nscr()
                  actf(rn[:], pbt[:, 0:256], AF.Sqrt, [bpb], [brn])
                  ts("dve", rn[:], rn[:], 1e-12, None, ALU.max, None, [brn], [brn])
                  recip(rn[:], rn[:], [brn], [brn])
                  tt("dve", kk[:], kk[:], rn[:], ALU.mult, [bkk, brn], [bkk])
                  if m == 0: ckpt("rw_kk")
                  cs, bcs = nscr()
                  scan(cs[:], scmask[:], sgw[:], 0.0, [b_scm, bsgw], [bcs])
                  cin, bcin = nscr()
                  cinv, bcinv = nscr()
                  cex, bcex = nscr()
                  actf(cin[:], cs[:], AF.Exp, [bcs], [bcin], scale=-DEC_K)
                  actf(cinv[:], cs[:], AF.Exp, [bcs], [bcinv], scale=DEC_K)
                  tt("pool", cex[:], cs[:], sgw[:], ALU.subtract, [bcs, bsgw], [bcex])
                  actf(cex[:], cex[:], AF.Exp, [bcex], [bcex], scale=-DEC_K)
                  import os as _os2
                  if _os2.environ.get("DBGV", "") != "K":
                      cp("dve", cL[pp][:].unsqueeze(2), cin[:].rearrange("p (u t) -> p u t", t=64)[:, :, 63:64], [bcin], [b_cL[pp]])
                  bAR = b_ARh; bBK = b_BKh
                  t1, bt1 = nscr()
                  tt("pool", t1[:], kk[:], aic[:], ALU.mult, [bkk, baic], [bt1])
                  kmod, bkmod = nscr()
                  ts("dve", kmod[:], aic[:], kacol[:, m:m + 1], omka[:, m:m + 1], ALU.mult, ALU.add, [baic, b_ka, b_omka], [bkmod])
                  tt("dve", kmod[:], kmod[:], kT, ALU.mult, [bkmod, bR3], [bkmod])
                  v3 = lambda a: a.rearrange("p (u t) -> p u t", t=64)
                  for h2 in range(2):
                      rws = slice(h2 * 64, (h2 + 1) * 64)
                      e1 = "pool" if h2 == 0 else "dve"
                      stt("dve", ARh[:, h2, :, 0, :], v3(kk[rws, :]), -1.0, v3(cex[rws, :]), ALU.mult, ALU.mult, [bkk, bcex], [bAR])
                      tt(e1, ARh[:, h2, :, 1, :], v3(R3[rws, 0, :]), v3(cin[rws, :]), ALU.mult, [bR3, bcin], [bAR])
                      tt("dve", BKh[:, h2, 0, :], t1[rws, :], cinv[rws, :], ALU.mult, [bt1, bcinv], [bBK])
                      tt(e1, BKh[:, h2, 1, :], kmod[rws, :], cinv[rws, :], ALU.mult, [bkmod, bcinv], [bBK])
                  prod, bprod = nscr()
                  prodb = prod[:].bitcast(BF16)[:, 0:256]
                  stt("dve", prodb, rT, rkcol[:, m:m + 1], kmod[:], ALU.mult, ALU.mult, [bR3, b_rk, bkmod], [bprod])
                  pbt, bpb = nbank()
                  for b in range(NB):
                      mm(pbt[:, b * 2:b * 2 + 2], prodb[:, b * TM:(b + 1) * TM], ind2[:], True, True, [bprod, b_ind2], [bpb])
                  cp("act", bon[pp][:], pbt[:, 0:4].rearrange("p (b h) -> p b h", b=NB), [bpb], [b_bon[pp]])
                  if m == 0: ckpt("rw_bonus")
                  pbt, bpb = nbank()
                  pv = pbt[:].bitcast(BF16)
                  for u in range(4):
                      for w2 in range(2):
                          for h2 in range(2):
                              c0_ = (u * 2 + w2) * 128 + h2 * 64
                              tr(pv[0:64, c0_:c0_ + 64], BKh[:, h2, w2, u * 64:(u + 1) * 64], ident[0:64, 0:64], [bBK, b_ident], [bpb])
                  cp("dve", tokBK[pp][:], pv[0:64, :].rearrange("p (u w f) -> p u w f", u=4, w=2), [bpb], [b_tokBK[pp]])
                  pbt, bpb = nbank()
                  pv = pbt[:].bitcast(BF16)
                  for u in range(4):
                      tr(pv[0:64, u * 128:(u + 1) * 128], vTb[pp][:, u * 64:(u + 1) * 64], ident[:], [b_vTb[pp], b_ident], [bpb])
                  for b in range(NB):
                      tr(pv[:, 512 + b * 128:512 + (b + 1) * 128], vTb[pp][:, b * TM:(b + 1) * TM], ident[:], [b_vTb[pp], b_ident], [bpb])
                  cp("act", tokV[pp][:], pv[0:64, 0:512].rearrange("p (u f) -> p u f", u=4), [bpb], [b_tokV[pp]])
                  cp("act", tokV2[pp][:], pv[:, 512:768].rearrange("p (b f) -> p b f", b=NB), [bpb], [b_tokV2[pp]])

                  if m == 0 and stop == "rw_tr": memset("dve", stat[:, 7:8], 1.0, [Buf("dummyM")])
                  if m == 0: ckpt("rw_tr")
                  ps1, bps1 = nbank()
                  ps1b, bps1b = nbank()
                  ps2, bps2 = nbank()
                  ps2b, bps2b = nbank()
                  ps3, bps3 = nbank()
                  import os as _os4
                  if _os4.environ.get("SWAPB", "") == "1":
                      ps1, bps1, ps3, bps3 = ps3, bps3, ps1, bps1
                  for h2 in range(2):
                      for u in range(4):
                          u8 = h2 * 4 + u
                          arhs = ARh[:, h2, u, :, :].rearrange("p a t -> p (a t)")
                          bl = BKh[:, h2, 0, u * 64:(u + 1) * 64]
                          kl = BKh[:, h2, 1, u * 64:(u + 1) * 64]
                          o1, bo1 = (ps1, bps1) if u8 < 4 else (ps1b, bps1b)
                          o2, bo2 = (ps2, bps2) if u8 < 4 else (ps2b, bps2b)
                          import os as _os3
                          _ns = int(_os3.environ.get("NSCORE", "99"))
                          _kinds = _os3.environ.get("SKIND", "123")
                          if u8 < _ns and "1" in _kinds:
                              mm(o1[0:64, (u8 % 4) * 128:(u8 % 4 + 1) * 128], bl, arhs, True, True, [bBK, bAR], [bo1])
                          if u8 < _ns and "2" in _kinds:
                              mm(o2[0:64, (u8 % 4) * 128:(u8 % 4 + 1) * 128], kl, arhs, True, True, [bBK, bAR], [bo2])
                          if u8 < _ns and "3" in _kinds:
                              mm(ps3[0:64, u8 * 64:(u8 + 1) * 64], ARh[:, h2, u, 0, :], bl, True, True, [bBK, bAR], [bps3])
                  if m == 0 and stop == "rw_scm": memset("dve", stat[:, 7:8], 1.0, [Buf("dummyM")])
                  if m == 0: ckpt("rw_scm")
                  mnb_b = mnb[:].unsqueeze(1).broadcast_to([64, 4, 128])
                  tt("dve", NBR[:, 0:4, :], ps1[0:64, :].rearrange("p (u f) -> p u f", u=4), mnb_b, ALU.mult, [bps1, b_mnb], [b_NBR])
                  tt("dve", NBR[:, 4:8, :], ps1b[0:64, :].rearrange("p (u f) -> p u f", u=4), mnb_b, ALU.mult, [bps1b, b_mnb], [b_NBR])
                  tt("dve", KAR[:, 0:4, :], ps2[0:64, :].rearrange("p (u f) -> p u f", u=4), mnb_b, ALU.mult, [bps2, b_mnb], [b_KAR])
                  tt("dve", KAR[:, 4:8, :], ps2b[0:64, :].rearrange("p (u f) -> p u f", u=4), mnb_b, ALU.mult, [bps2b, b_mnb], [b_KAR])
                  tt("dve", Qm[0][:], ps3[0:64, :].rearrange("p (u f) -> p u f", u=8), mq[:].unsqueeze(1).broadcast_to([64, 8, 64]), ALU.mult, [bps3, b_mq], [b_Qm[0]])
                  if m == 0: ckpt("rw_sc")
                  Pcur, bPcur = NBR[:, :, 0:64], b_NBR
                  qi = 0
                  ai = 0
                  import os as _os
                  _v = _os.environ.get("DBGV", "")
                  if _v == "A":
                      tt("dve", Am[0][:], NBR[:, :, 0:64], identf[0:64, 0:64].unsqueeze(1).broadcast_to([64, 8, 64]), ALU.add, [b_NBR, b_identf], [b_Am[0]])
                  elif _v == "B":
                      memset("dve", Am[0][:], 1.0, [b_Am[0]])
                      cp("act", Amb[:], Am[0][:], [b_Am[0]], [b_Amb])
                  elif _v == "C":
                      memset("dve", Am[0][:], 1.0, [b_Am[0]])
                  elif _v == "G":
                      memset("dve", stat[:, 7:8], 1.0, [Buf("dummyG")])
                  elif _v == "G2":
                      memset("dve", stat[:, 7:8], 1.0, [Buf("dummyG")])
                      memset("dve", stat[:, 6:7], 1.0, [Buf("dummyG2")])
                  elif _v == "GP":
                      memset("pool", stat[:, 7:8], 1.0, [Buf("dummyG")])
                  elif _v == "K":
                      memset("dve", stat[:, 7:8], 1.0, [Buf("dummyG")])
                  elif _v == "H":
                      pass
                  elif _v == "D":
                      memset("dve", mixT[:], 1.0, [b_mixT])
                  elif _v == "E":
                      memset("dve", Am[1][:], 1.0, [b_Am[1]])
                  elif _v == "F":
                      memset("dve", Qm[1][:], 1.0, [b_Qm[1]])
                  else:
                      tt("dve", Am[0][:], NBR[:, :, 0:64], identf[0:64, 0:64].unsqueeze(1).broadcast_to([64, 8, 64]), ALU.add, [b_NBR, b_identf], [b_Am[0]])
                      cp("act", Amb[:], Am[0][:], [b_Am[0]], [b_Amb])
                  if m == 0: ckpt("rw_c0")
                  for lvl in range(1, 6):
                      Qc, bQc = Qm[qi], b_Qm[qi]
                      Qn, bQn = Qm[1 - qi], b_Qm[1 - qi]
                      psq, bpsq = nbank()
                      for u8 in range(8):
                          mm(psq[0:64, u8 * 64:(u8 + 1) * 64], Pcur[:, u8, :], Qc[:, u8, :], True, True, [bPcur, bQc], [bpsq])
                      if lvl < 5:
                          psp, bpsp = nbank()
                          for u8 in range(8):
                              mm(psp[0:64, u8 * 64:(u8 + 1) * 64], Qc[:, u8, :], Pcur[:, u8, :], True, True, [bPcur, bQc], [bpsp])
                      cp("act", Qn[:], psq[0:64, :].rearrange("p (u f) -> p u f", u=8), [bpsq], [bQn])
                      if lvl < 5:
                          Pn, bPn = Pm[lvl % 2], b_Pm[lvl % 2]
                          cp("dve", Pn[:], psp[0:64, :].rearrange("p (u f) -> p u f", u=8), [bpsp], [bPn])
                          Pcur, bPcur = Pn[:], bPn
                      qi = 1 - qi
                      psa, bpsa = nbank()
                      for u8 in range(8):
                          mm(psa[0:64, u8 * 64:(u8 + 1) * 64], Qn[:, u8, :], Amb[:, u8, :], True, True, [bQn, b_Amb], [bpsa])
                      tt("dve", Am[1 - ai][:], psa[0:64, :].rearrange("p (u f) -> p u f", u=8), Am[ai][:], ALU.add, [bpsa, b_Am[ai]], [b_Am[1 - ai]])
                      ai = 1 - ai
                      cp("act", Amb[:], Am[ai][:], [b_Am[ai]], [b_Amb])
                      if m == 0 and lvl == 1: ckpt("rw_c1")
                  if m == 0: ckpt("rw_chain")
                  for c in range(2):
                      psx, bpsx = nbank()
                      for b in range(NB):
                          for h2 in range(2):
                              rows = slice(h2 * 64, (h2 + 1) * 64)
                              u = b * 2 + c
                              u8 = h2 * 4 + u
                              o = psx[0:64, (b * 2 + h2) * 64:(b * 2 + h2 + 1) * 64]
                              mm(o, ARh[:, h2, u, 0, :], STb[:, m * 2 + h2, b, :], True, False, [bAR, b_STb[m]], [bpsx])
                              mm(o, KAR[:, u8, 0:64], tokV[pp][:, u, h2 * 64:(h2 + 1) * 64], False, True, [b_KAR, b_tokV[pp]], [bpsx])
                      cp("act", Xs[:], psx[0:64, 0:256].rearrange("p (u f) -> p u f", u=4), [bpsx], [b_Xs])
                      psu, bpsu = nbank()
                      for b in range(NB):
                          for h2 in range(2):
                              u8 = h2 * 4 + b * 2 + c
                              mm(psu[0:64, (b * 2 + h2) * 64:(b * 2 + h2 + 1) * 64], Amb[:, u8, :], Xs[:, b * 2 + h2, :], True, True, [b_Amb, b_Xs], [bpsu])
                      cp("dve", Us[:], psu[0:64, 0:256].rearrange("p (b f) -> p b f", b=NB), [bpsu], [b_Us])
                      psy, bpsy = nbank()
                      for b in range(NB):
                          for h2 in range(2):
                              rows = slice(h2 * 64, (h2 + 1) * 64)
                              u = b * 2 + c
                              u8 = h2 * 4 + u
                              o = psy[0:64, (b * 2 + h2) * 64:(b * 2 + h2 + 1) * 64]
                              mm(o, ARh[:, h2, u, 1, :], STb[:, m * 2 + h2, b, :], True, False, [bAR, b_STb[m]], [bpsy])
                              mm(o, NBR[:, u8, 64:128], Us[:, b, h2 * 64:(h2 + 1) * 64], False, False, [b_NBR, b_Us], [bpsy])
                              mm(o, KAR[:, u8, 64:128], tokV[pp][:, u, h2 * 64:(h2 + 1) * 64], False, True, [b_KAR, b_tokV[pp]], [bpsy])
                      cp("act", Ytok[c * 64:(c + 1) * 64, :, :], psy[0:64, 0:256].rearrange("p (b f) -> p b f", b=NB), [bpsy], [b_Ytok])
                      pss, bpss = nbank()
                      for b in range(NB):
                          u = b * 2 + c
                          o = pss[:, b * 128:(b + 1) * 128]
                          mm(o, tokBK[pp][:, u, 0, :], Us[:, b, :], True, False, [b_tokBK[pp], b_Us], [bpss])
                          mm(o, tokBK[pp][:, u, 1, :], tokV[pp][:, u, :], False, True, [b_tokBK[pp], b_tokV[pp]], [bpss])
                      for b in range(NB):
                          u = b * 2 + c
                          for h2 in range(2):
                              rows = slice(h2 * 64, (h2 + 1) * 64)
                              tt("dve", ST[rows, m, b, :], pss[rows, b * 128 + h2 * 64:b * 128 + (h2 + 1) * 64], ST[rows, m, b, :], ALU.add, [bpss, b_ST[m]], [b_ST[m]])
                          ts("dve", ST[:, m, b, :], ST[:, m, b, :], cL[pp][:, u:u + 1], None, ALU.mult, None, [b_ST[m], b_cL[pp]], [b_ST[m]])
                      cp("act", STb[:, m * 2, :, :], ST[0:64, m, :, :], [b_ST[m]], [b_STb[m]])
                      cp("dve", STb[:, m * 2 + 1, :, :], ST[64:128, m, :, :], [b_ST[m]], [b_STb[m]])
                  if it == 0 and m == 0:
                      dbg_dump("Ytok", Ytok[:], [128, NB, 128], b_Ytok)
                  if m == 0: ckpt("rw_seq")
                  Y4 = Ytok[:].rearrange("p b (h i) -> p (b h) i", h=2)
                  G0 = gnt[0][:].rearrange("p b (h i) -> p (b h) i", h=2)
                  G1 = gnt[1][:].rearrange("p b (h i) -> p (b h) i", h=2)
                  rsum(gst[:, 0:4], Y4, [b_Ytok], [b_gst])
                  ts("dve", gst[:, 0:4], gst[:, 0:4], -1.0 / 64, None, ALU.mult, None, [b_gst], [b_gst])
                  tt("dve", G0, Y4, gst[:, 0:4].unsqueeze(2).broadcast_to([128, 4, 64]), ALU.add, [b_Ytok, b_gst], [b_gnt[0]])
                  tt("pool", G1, G0, G0, ALU.mult, [b_gnt[0]], [b_gnt[1]])
                  rsum(gst[:, 4:8], G1, [b_gnt[1]], [b_gst])
                  actf(gst[:, 4:8], gst[:, 4:8], AF.Sqrt, [b_gst], [b_gst], bias=GN_EPS, scale=1.0 / 64)
                  recip(gst[:, 4:8], gst[:, 4:8], [b_gst], [b_gst])
                  tt("dve", G0, G0, gst[:, 4:8].unsqueeze(2).broadcast_to([128, 4, 64]), ALU.mult, [b_gnt[0], b_gst], [b_gnt[0]])
                  lw = lnw[:, m * 128:(m + 1) * 128].unsqueeze(1).broadcast_to([128, NB, 128])
                  lb = lnb[:, m * 128:(m + 1) * 128].unsqueeze(1).broadcast_to([128, NB, 128])
                  tt("dve", gnt[0][:], gnt[0][:], lw, ALU.mult, [b_gnt[0], b_lnw], [b_gnt[0]])
                  tt("pool", gnt[0][:], gnt[0][:], lb, ALU.add, [b_gnt[0], b_lnb], [b_gnt[0]])
                  tt("dve", G1, tokV2[pp][:].rearrange("p b (h i) -> p (b h) i", h=2),
                     bon[pp][:].rearrange("p b h -> p (b h)").unsqueeze(2).broadcast_to([128, 4, 64]), ALU.mult, [b_tokV2[pp], b_bon[pp]], [b_gnt[1]])
                  tt("pool", gnt[0][:], gnt[0][:], gnt[1][:], ALU.add, [b_gnt[0], b_gnt[1]], [b_gnt[0]])
                  tt("dve", ygtok[:], gnt[0][:], gtok[pp][:], ALU.mult, [b_gnt[0], b_gtok[pp]], [b_ygtok])
                  pbt, bpb = nbank()
                  pv = pbt[:].bitcast(BF16)
                  for b in range(NB):
                      tr(pv[:, b * 128:(b + 1) * 128], ygtok[:, b, :], ident[:], [b_ygtok, b_ident], [bpb])
                  cp("act", ygT[:, m, :], pv[:, 0:256], [bpb], [b_ygT])
              if it == 0:
                  dbg_dump("ygT", ygT[:], [128, 8, 256], b_ygT, BF16)

              ckpt("rwkv")
              w_u = [None, None, None, None]
              for j in range(4):
                  w_u[j] = load_w(3360 + j * 256, 256)
                  for jj in range(2):
                      mb = j * 2 + jj
                      pbt, bpb = nbank()
                      proj(w_u[j][0], w_u[j][1], jj * 128, 128, pbt[:, 0:256], bpb)
                      cp("dve", uT[:, mb, :], pbt[:, 0:256], [bpb], [b_uT])
              for m8 in range(8):
                  X = xs5[m8 % 2]; bX = b_xs5[m8 % 2]
                  for tq in range(4):
                      t = m8 * 4 + tq
                      po = 64 * (tq // 2)
                      scr_reset()
                      pbt, bpb = nbank()
                      for ri in range(2):
                          mm(pbt[:, ri * 256:(ri + 1) * 256], Bl[:, t, ri, :], uT[:, m8, :], True, True, [b_Bl, b_uT], [bpb])
                      tC = tabC[:, t, :].unsqueeze(1).broadcast_to([128, 4, 128])
                      tS = tabS[:, t, :].unsqueeze(1).broadcast_to([128, 4, 128])
                      a1, ba1 = nscr(); a2, ba2 = nscr()
                      pc4 = pbt[:, :].rearrange("p (q t) -> p q t", t=128)
                      a3, ba3 = nscr(); a4, ba4 = nscr()
                      tt("dve", a1[:].rearrange("p (q t) -> p q t", t=128), pc4[:, 0:2, :], tC[:, 0:2, :], ALU.mult, [bpb, b_tabC], [ba1])
                      tt("dve", a2[:].rearrange("p (q t) -> p q t", t=128), pc4[:, 2:4, :], tS[:, 0:2, :], ALU.mult, [bpb, b_tabS], [ba2])
                      tt("dve", a3[:].rearrange("p (q t) -> p q t", t=128), pc4[:, 2:4, :], tC[:, 0:2, :], ALU.mult, [bpb, b_tabC], [ba3])
                      tt("dve", a4[:].rearrange("p (q t) -> p q t", t=128), pc4[:, 0:2, :], tS[:, 0:2, :], ALU.mult, [bpb, b_tabS], [ba4])
                      tt("pool", a1[:], a1[:], a2[:], ALU.add, [ba1, ba2], [ba1])
                      tt("pool", a3[:], a3[:], a4[:], ALU.subtract, [ba3, ba4], [ba3])
                      for b in range(NB):
                          scan(a2[:, b * TM:(b + 1) * TM], rho[:, t:t + 1].broadcast_to([128, TM]), a1[:, b * TM:(b + 1) * TM], s5c[:, t, 0, b:b + 1], [b_rho, ba1, b_s5c[t]], [ba2])
                          scan(a4[:, b * TM:(b + 1) * TM], rho[:, t:t + 1].broadcast_to([128, TM]), a3[:, b * TM:(b + 1) * TM], s5c[:, t, 1, b:b + 1], [b_rho, ba3, b_s5c[t]], [ba4])
                      w_re3 = a2[:].rearrange("p (q t) -> p q t", t=128)
                      w_im3 = a4[:].rearrange("p (q t) -> p q t", t=128)
                      tt("dve", a1[:].rearrange("p (q t) -> p q t", t=128), w_re3, tC[:, 0:2, :], ALU.mult, [ba2, b_tabC], [ba1])
                      tt("pool", a3[:].rearrange("p (q t) -> p q t", t=128), w_im3, tS[:, 0:2, :], ALU.mult, [ba4, b_tabS], [ba3])
                      a5, ba5 = nscr(); a6, ba6 = nscr()
                      tt("dve", a5[:].rearrange("p (q t) -> p q t", t=128), w_re3, tS[:, 0:2, :], ALU.mult, [ba2, b_tabS], [ba5])
                      tt("pool", a6[:].rearrange("p (q t) -> p q t", t=128), w_im3, tC[:, 0:2, :], ALU.mult, [ba4, b_tabC], [ba6])
                      tt("dve", X[:, tq, 0, :], a1[:], a3[:], ALU.subtract, [ba1, ba3], [bX])
                      tt("pool", X[:, tq, 1, :], a5[:], a6[:], ALU.add, [ba5, ba6], [bX])
                      l1 = a1[:].rearrange("p (b t) -> p b t", b=NB)[:, :, TM - 1]
                      l3 = a3[:].rearrange("p (b t) -> p b t", b=NB)[:, :, TM - 1]
                      l5 = a5[:].rearrange("p (b t) -> p b t", b=NB)[:, :, TM - 1]
                      l6 = a6[:].rearrange("p (b t) -> p b t", b=NB)[:, :, TM - 1]
                      tt("dve", s5c[:, t, 0, :], l1, l3, ALU.subtract, [ba1, ba3], [b_s5c[t]])
                      tt("dve", s5c[:, t, 1, :], l5, l6, ALU.add, [ba5, ba6], [b_s5c[t]])
                  pbt, bpb = nbank()
                  for tq in range(4):
                      t = m8 * 4 + tq
                      for ri in range(2):
                          mm(pbt[:, 0:256], Cl[:, t, ri, :], X[:, tq, ri, :], tq == 0 and ri == 0, tq == 3 and ri == 1, [b_Cl, bX], [bpb])
                  yy, byy = nscr(10)
                  y2, by2 = nscr(11)
                  stt("dve", yy[:], uT[:, m8, :], dcol[:, m8:m8 + 1], pbt[:, 0:256], ALU.mult, ALU.add, [b_uT, b_dc, bpb], [byy])
                  tt("pool", y2[:], yy[:], yy[:], ALU.mult, [byy], [by2])
                  ts("dve", y2[:], y2[:], 0.044715, 1.0, ALU.mult, ALU.add, [by2], [by2])
                  tt("pool", y2[:], y2[:], yy[:], ALU.mult, [by2, byy], [by2])
                  actf(y2[:], y2[:], AF.Sigmoid, [by2], [by2], scale=1.5957691216057308)
                  tt("dve", zT[:, m8, :], y2[:], yy[:], ALU.mult, [by2, byy], [b_zT])
              if it == 0:
                  dbg_dump("zT", zT[:], [128, 8, 256], b_zT, BF16)

              ckpt("s5")
              wro_v = wro_bf.rearrange("(kc p) c -> p kc c", p=128)
              wgv_v = wgv_bf.rearrange("(kc p) c -> p kc c", p=128)
              wgg_v = wgg_bf.rearrange("(kc p) c -> p kc c", p=128)
              for f2 in range(8):
                  wga = load_w(4384 + f2 * 256, 256)
                  wgb = load_w(6432 + f2 * 256, 256)
                  for ff in range(2):
                      f = f2 * 2 + ff
                      wr1, bwr1 = wsmslot()
                      S.dma("sp", wr1[:], wro_v[:, :, f * 128:(f + 1) * 128], r=[b_mixw], w=[bwr1])
                      wr2, bwr2 = wsmslot()
                      S.dma("sp", wr2[:], wgv_v[:, :, f * 128:(f + 1) * 128], r=[b_mixw], w=[bwr2])
                      wr3, bwr3 = wsmslot()
                      S.dma("sp", wr3[:], wgg_v[:, :, f * 128:(f + 1) * 128], r=[b_mixw], w=[bwr3])
                      scr_reset()
                      pg, bpg = nbank()
                      proj(wga[0], wga[1], ff * 128, 128, pg[:, 0:256], bpg)
                      proj(wgb[0], wgb[1], ff * 128, 128, pg[:, 256:512], bpg)
                      py, bpy = nbank()
                      for kc in range(8):
                          mm(py[:, 0:256], wr1[:, kc, :], ygT[:, kc, :], kc == 0, kc == 7, [bwr1, b_ygT], [bpy])
                      for kc in range(8):
                          mm(py[:, 256:512], wr2[:, kc, :], zT[:, kc, :], kc == 0, kc == 7, [bwr2, b_zT], [bpy])
                      pq, bpq = nbank()
                      for kc in range(8):
                          mm(pq[:, 0:256], wr3[:, kc, :], zT[:, kc, :], kc == 0, kc == 7, [bwr3, b_zT], [bpq])
                      sa, bsa = nscr(); sb_, bsb_ = nscr(); sg_, bsg_ = nscr()
                      actf(sa[:], pg[:, 0:256], AF.Sigmoid, [bpg], [bsa])
                      actf(sb_[:], pg[:, 256:512], AF.Sigmoid, [bpg], [bsb_])
                      actf(sg_[:], pq[:, 0:256], AF.Sigmoid, [bpq], [bsg_])
                      tt("dve", sa[:], py[:, 0:256], sa[:], ALU.mult, [bpy, bsa], [bsa])
                      tt("dve", sg_[:], py[:, 256:512], sg_[:], ALU.mult, [bpy, bsg_], [bsg_])
                      tt("pool", sg_[:], sg_[:], sb_[:], ALU.mult, [bsg_, bsb_], [bsg_])
                      tt("pool", mixT[:, f, :], sa[:], sg_[:], ALU.add, [bsa, bsg_], [b_mixT])
              if it == 0:
                  dbg_dump("mixT", mixT[:], [128, 16, 256], b_mixT, BF16)

              ckpt("merge")
              wo_v = wo_bf.rearrange("(kc p) c -> p kc c", p=128)
              for cb in range(8):
                  w_, bw = wslot()
                  S.dma("sp", w_[:], wo_v[:, :, cb * 256:(cb + 1) * 256], r=[b_mixw], w=[bw])
                  for b in range(NB):
                      i2 = (cb * NB + b) % 2
                      S.dma("sp", xres[i2][:], xin[b, t0:t0 + TM, cb * 256:(cb + 1) * 256], r=[], w=[b_xres[i2]])
                      pbt, bpb = nbank()
                      for kc in range(16):
                          mm(pbt[:, 0:256], mixT[:, kc, b * TM:(b + 1) * TM], w_[:, kc, :], kc == 0, kc == 15, [b_mixT, bw], [bpb])
                      tt("dve", x2o[i2][:], pbt[:, 0:256], xres[i2][:], ALU.add, [bpb, b_xres[i2]], [b_x2o[i2]])
                      S.dma("sp", (x2_ext[b, t0:t0 + TM, cb * 256:(cb + 1) * 256] if split == "M" else x2_dr[b * SEQ + t0:b * SEQ + t0 + TM, cb * 256:(cb + 1) * 256]), x2o[i2][:], r=[b_x2o[i2]], w=[], chan=b_x2st[i2])
          if split == "M":
              S.dma("sp", st_out, ST[:].rearrange("p a b c -> p (a b c)"), r=b_ST, w=[], chan=Buf("ch_sto"))
              S.dma("sp", s5c_out, s5c[:].rearrange("p a b c -> p (a b c)"), r=b_s5c, w=[], chan=Buf("ch_s5o"))
              S.dma("sp", carry_out, carry[:].rearrange("p a b -> p (a b)"), r=b_carry, w=[], chan=Buf("ch_co"))
          ckpt("wout")
          S.barrier()
          S.flush()

    except _SkipM:
        pass
    except _Stop:
        raise
    if split == "M":
        top.close()
        return nc, dbg_out, S
    with ExitStack() as pf:
        g2col, b_g2 = colvec(pf, "g2col", P["norm_ffn_g"], 16)
        gF, b_gF = bcast_rows(pf, "gF", P["norm_final_g"], D)
        rbias = sb(pf, "rbias", [128, 36], F32); b_rbias = Buf("rbias")
        b_rbias2 = Buf("rbias2")
        S.dma("sp", rbias[:, 0:4], P["router_group_b"].partition_broadcast(128), r=[], w=[b_rbias])
        S.dma("sp", rbias[:, 4:36], P["router_expert_b"].partition_broadcast(128), r=[], w=[b_rbias2])
        wr = sb(pf, "wr", [128, 16, 36], BF16); b_wr = Buf("wr"); b_wr2 = Buf("wr2")
        S.dma("sp", wr[:, :, 0:4], wrg_bf.rearrange("(kc p) c -> p kc c", p=128), r=[b_mixw], w=[b_wr], allow_slow_non_contiguous=True)
        S.dma("sp", wr[:, :, 4:36], wre_bf.rearrange("(kc p) c -> p kc c", p=128), r=[b_mixw], w=[b_wr2], allow_slow_non_contiguous=True)
        acc = sb(pf, "acc", [128, 4, D], F32); b_acc = [Buf("acc%d" % q) for q in range(4)]
        xsf = sb(pf, "xsf", [128, D], BF16); b_xsf = Buf("xsf")
        h2T = sb(pf, "h2T", [128, 16, TF], BF16); b_h2T = Buf("h2T")
        fst = sb(pf, "fst", [128, 4, 4], F32); b_fst = [Buf("fst%d" % q) for q in range(4)]
        gates = sb(pf, "gates", [128, 4, NE], F32); b_gates = [Buf("gates%d" % q) for q in range(4)]
        rt = [sb(pf, "rt%d" % i, [128, 36], F32) for i in range(6)]; b_rt = [Buf("rt%d" % i) for i in range(6)]
        rs = sb(pf, "rs", [128, 8], F32); b_rs = Buf("rs")
        Wg = [sb(pf, "Wg%d" % i, [128, 16, DE], BF16) for i in range(2)]; b_Wg = [Buf("Wg%d" % i) for i in range(2)]
        Wu = [sb(pf, "Wu%d" % i, [128, 16, DE], BF16) for i in range(2)]; b_Wu = [Buf("Wu%d" % i) for i in range(2)]
        Wd = [sb(pf, "Wd%d" % i, [128, 4, D], BF16) for i in range(2)]; b_Wd = [Buf("Wd%d" % i) for i in range(2)]
        sgt = [sb(pf, "sgt%d" % i, [128, TF], F32) for i in range(2)]; b_sgt = [Buf("sgt%d" % i) for i in range(2)]
        hid = [sb(pf, "hid%d" % i, [128, 4, TF], BF16) for i in range(2)]; b_hid = [Buf("hid%d" % i) for i in range(2)]
        b_yst = [Buf("yst%d" % q) for q in range(4)]
        b_ydr = Buf("ydr")
        wg_v = wg_bf.rearrange("e (kc p) f -> e p kc f", p=128)
        wu_v = wu_bf.rearrange("e (kc p) f -> e p kc f", p=128)
        wd_v = wd_bf.rearrange("e (fc p) c -> e p fc c", p=128)

        for jt in range(ntf):
            r0 = jt * TF
            pstate["i"] = 0
            for q in range(4):
                S.dma("sp", acc[:, q, :], x2_dr[r0 + q * 128:r0 + (q + 1) * 128, :], r=[], w=[b_acc[q]])
                actf(xsf[:], acc[:, q, :], AF.Square, [b_acc[q]], [b_xsf, b_fst[q]], accum=fst[:, q, 0:1])
                actf(fst[:, q, 1:2], fst[:, q, 0:1], AF.Sqrt, [b_fst[q]], [b_fst[q]], bias=RMS_EPS, scale=1.0 / D)
                recip(fst[:, q, 2:3], fst[:, q, 1:2], [b_fst[q]], [b_fst[q]])
                actf(xsf[:], acc[:, q, :], AF.Identity, [b_acc[q], b_fst[q]], [b_xsf], scale=fst[:, q, 2:3])
                for half in range(2):
                    pbt, bpb = nbank()
                    pv = pbt[:].bitcast(BF16)
                    for k8 in range(8):
                        kc = half * 8 + k8
                        tr(pv[:, k8 * 128:(k8 + 1) * 128], xsf[:, kc * 128:(kc + 1) * 128], ident[:], [b_xsf, b_ident], [bpb])
                    tt("dve", h2T[:, half * 8:half * 8 + 8, q * 128:(q + 1) * 128], pv[:, :].rearrange("p (k t) -> p k t", k=8),
                       g2col[:, half * 8:half * 8 + 8].unsqueeze(2).broadcast_to([128, 8, 128]), ALU.mult, [bpb, b_g2], [b_h2T])
                pbt, bpb = nbank()
                for kc in range(16):
                    mm(pbt[:, 0:36], h2T[:, kc, q * 128:(q + 1) * 128], wr[:, kc, :], kc == 0, kc == 15, [b_h2T, b_wr, b_wr2], [bpb])
                lg, blg = rt[0], b_rt[0]
                tt("dve", lg[:], pbt[:, 0:36], rbias[:], ALU.add, [bpb, b_rbias, b_rbias2], [blg])
                rmax(rs[:, 0:1], lg[:, 0:4], [blg], [b_rs])
                oh, boh = rt[1], b_rt[1]
                ts("dve", oh[:, 0:4], lg[:, 0:4], rs[:, 0:1], None, ALU.is_ge, None, [blg, b_rs], [boh])
                ex, bex_ = rt[2], b_rt[2]
                ts("dve", ex[:, 0:4], lg[:, 0:4], rs[:, 0:1], None, ALU.subtract, None, [blg, b_rs], [bex_])
                actf(ex[:, 0:4], ex[:, 0:4], AF.Exp, [bex_], [bex_])
                rsum(rs[:, 1:2], ex[:, 0:4], [bex_], [b_rs])
                recip(rs[:, 2:3], rs[:, 1:2], [b_rs], [b_rs])
                msk, bmsk = rt[3], b_rt[3]
                ts("dve", oh[:, 4:8], oh[:, 0:4], -1.0, 1e30, ALU.add, ALU.mult, [boh], [boh])
                tt("dve", msk[:, 0:32].rearrange("p (g e) -> p g e", g=4), lg[:, 4:36].rearrange("p (g e) -> p g e", g=4),
                   oh[:, 4:8].unsqueeze(2).broadcast_to([128, 4, 8]), ALU.add, [blg, boh], [bmsk])
                rmax(rs[:, 3:4], msk[:, 0:32], [bmsk], [b_rs])
                m1, bm1 = rt[4], b_rt[4]
                ts("dve", m1[:, 0:32], msk[:, 0:32], rs[:, 3:4], None, ALU.is_ge, None, [bmsk, b_rs], [bm1])
                stt("dve", msk[:, 0:32], m1[:, 0:32], -1e30, msk[:, 0:32], ALU.mult, ALU.add, [bm1, bmsk], [bmsk])
                rmax(rs[:, 4:5], msk[:, 0:32], [bmsk], [b_rs])
                m2, bm2 = rt[5], b_rt[5]
                ts("dve", m2[:, 0:32], msk[:, 0:32], rs[:, 4:5], None, ALU.is_ge, None, [bmsk, b_rs], [bm2])
                tt("dve", rs[:, 5:6], rs[:, 4:5], rs[:, 3:4], ALU.subtract, [b_rs], [b_rs])
                actf(rs[:, 5:6], rs[:, 5:6], AF.Exp, [b_rs], [b_rs])
                ts("dve", rs[:, 5:6], rs[:, 5:6], 1.0, None, ALU.add, None, [b_rs], [b_rs])
                recip(rs[:, 5:6], rs[:, 5:6], [b_rs], [b_rs])
                ts("dve", rs[:, 6:7], rs[:, 5:6], -1.0, 1.0, ALU.mult, ALU.add, [b_rs], [b_rs])
                tt("dve", rs[:, 5:6], rs[:, 5:6], rs[:, 2:3], ALU.mult, [b_rs], [b_rs])
                tt("dve", rs[:, 6:7], rs[:, 6:7], rs[:, 2:3], ALU.mult, [b_rs], [b_rs])
                ts("dve", m1[:, 0:32], m1[:, 0:32], rs[:, 5:6], None, ALU.mult, None, [bm1, b_rs], [bm1])
                stt("dve", gates[:, q, :], m2[:, 0:32], rs[:, 6:7], m1[:, 0:32], ALU.mult, ALU.add, [bm2, b_rs, bm1], [b_gates[q]])
            if jt == 0:
                dbg_dump("gates", gates[:], [128, 4, NE], b_gates[3])
            S.pe_serial = False
            for e in range(NE):
                i2 = e % 2
                bmw = b_moew[e // 8]
                S.dma("sp", Wg[i2][:], wg_v[e], r=[bmw], w=[b_Wg[i2]])
                S.dma("sp", Wu[i2][:], wu_v[e], r=[bmw], w=[b_Wu[i2]])
                S.dma("sp", Wd[i2][:], wd_v[e], r=[bmw], w=[b_Wd[i2]])
                H = hid[i2]; bH = b_hid[i2]
                for fb in range(4):
                    pg, bpg = nbank()
                    for kc in range(16):
                        mm(pg[:, :], Wg[i2][:, kc, fb * 128:(fb + 1) * 128], h2T[:, kc, :], kc == 0, kc == 15, [b_Wg[i2], b_h2T], [bpg])
                    pu, bpu = nbank()
                    for kc in range(16):
                        mm(pu[:, :], Wu[i2][:, kc, fb * 128:(fb + 1) * 128], h2T[:, kc, :], kc == 0, kc == 15, [b_Wu[i2], b_h2T], [bpu])
                    sg2 = sgt[fb % 2]; bsg2 = b_sgt[fb % 2]
                    actf(sg2[:], pg[:, :], AF.Silu, [bpg], [bsg2])
                    tt("dve", H[:, fb, :], pu[:, :], sg2[:], ALU.mult, [bpu, bsg2], [bH])
                for q in range(4):
                    for cb in range(4):
                        pd, bpd = nbank()
                        for fc in range(4):
                            mm(pd[:, :], H[:, fc, q * 128:(q + 1) * 128], Wd[i2][:, fc, cb * 512:(cb + 1) * 512], fc == 0, fc == 3, [bH, b_Wd[i2]], [bpd])
                        stt("dve", acc[:, q, cb * 512:(cb + 1) * 512], pd[:, :], gates[:, q, e:e + 1], acc[:, q, cb * 512:(cb + 1) * 512],
                            ALU.mult, ALU.add, [bpd, b_gates[q], b_acc[q]], [b_acc[q]])
            S.pe_serial = True
            for q in range(4):
                actf(xsf[:], acc[:, q, :], AF.Square, [b_acc[q]], [b_xsf, b_fst[q]], accum=fst[:, q, 0:1])
                actf(fst[:, q, 1:2], fst[:, q, 0:1], AF.Sqrt, [b_fst[q]], [b_fst[q]], bias=RMS_EPS, scale=1.0 / D)
                recip(fst[:, q, 2:3], fst[:, q, 1:2], [b_fst[q]], [b_fst[q]])
                stt("dve", acc[:, q, :], acc[:, q, :], fst[:, q, 2:3], gF[:], ALU.mult, ALU.mult, [b_acc[q], b_fst[q], b_gF], [b_acc[q]])
                S.dma("sp", yout[r0 + q * 128:r0 + (q + 1) * 128, :], acc[:, q, :], r=[b_acc[q]], w=[], chan=b_yst[q])
        S.wait_events("sp", S.all_events())
        S.flush()
    top.close()
    return nc, dbg_out, S


_CACHE = {}


def kernel(**inputs):
    x = np.ascontiguousarray(np.asarray(inputs["x"], dtype=np.float32))
    if "nc" not in _CACHE:
        _CACHE["nc"] = build_nc()[0]
    nc = _CACHE["nc"]
    shared = {}
    for name, shp in PARAM_SHAPES:
        shared[name] = np.ascontiguousarray(np.asarray(inputs[name], dtype=np.float32).reshape(shp))
    in_maps = []
    for c in range(8):
        m = dict(shared)
        m["x"] = x[2 * c:2 * c + 2]
        in_maps.append(m)
    res = run_bass_kernel_spmd(nc, in_maps, core_ids=list(range(8)))
    out = np.empty((16, SEQ, D), np.float32)
    for c in range(8):
        out[2 * c:2 * c + 2] = np.asarray(res.results[c]["y"]).reshape(2, SEQ, D)
    return out
```
